# Optimizing a Trainium2 kernel written in Bass

```python
import math, functools
import jax, jax.numpy as jnp
from jax import lax
import numpy as np

D_MODEL = 1024
BATCH = 2
SEQ = 8192
DEPTH = 4

BLOCK = 128
ROPE_THETA = 10000.0
RMS_EPS = 1e-6
NEG_INF = -1e30
GRID_W = 64

N_HEADS_A = 8
N_KV_A = 2
HEAD_DIM_A = 64
GROUP_A = N_HEADS_A // N_KV_A
WINDOW = 128
A_Q = N_HEADS_A * HEAD_DIM_A
A_KV = N_KV_A * HEAD_DIM_A

POOL_WINDOWS = (2, 4, 8, 16)
N_POOL_GROUPS = 4
POOL_WIDTH = D_MODEL // 2
POOL_GROUP = POOL_WIDTH // N_POOL_GROUPS

EVEN_IN = A_Q + 2 * A_KV + POOL_WIDTH
EVEN_OUT = A_Q + POOL_WIDTH

N_HEADS_C = 8
N_KV_C = 2
HEAD_DIM_C = 128
GROUP_C = N_HEADS_C // N_KV_C
C_Q = N_HEADS_C * HEAD_DIM_C
C_KV = N_KV_C * HEAD_DIM_C
ODD_IN = C_Q + 2 * C_KV
AXIAL_DIM = HEAD_DIM_C // 2

N_EXPERTS = 16
EXPERT_FF = 2048
CAPACITY_FACTOR = 2

N_EVEN = (DEPTH + 1) // 2
N_ODD = DEPTH // 2

kernel_name = "hybrid_swa_pool_axialattn_ecmoe_encoder"


def rms_norm(x, g):
    xf = x.astype(jnp.float32)
    y = xf * lax.rsqrt(jnp.mean(xf * xf, axis=-1, keepdims=True) + RMS_EPS)
    return (y * g.astype(jnp.float32)).astype(x.dtype)


def rope_cos_sin(pos, dim):
    inv = ROPE_THETA ** (-jnp.arange(0, dim, 2, dtype=jnp.float32) / dim)
    ang = pos.astype(jnp.float32)[:, None] * inv[None, :]
    return jnp.cos(ang), jnp.sin(ang)


def apply_rope(x, cos, sin):
    half = x.shape[-1] // 2
    x1, x2 = x[..., :half], x[..., half:]
    c = cos[None, :, None, :].astype(x.dtype)
    s = sin[None, :, None, :].astype(x.dtype)
    return jnp.concatenate([x1 * c - x2 * s, x1 * s + x2 * c], axis=-1)


def apply_axial_rope(x, cos_r, sin_r, cos_c, sin_c):
    xr, xc = x[..., :AXIAL_DIM], x[..., AXIAL_DIM:]
    return jnp.concatenate([apply_rope(xr, cos_r, sin_r), apply_rope(xc, cos_c, sin_c)], axis=-1)


def windowed_sink_attention(q, k, v, sink):
    b, s, _, _ = q.shape
    nb = s // BLOCK
    qb = q.reshape(b, nb, BLOCK, N_KV_A, GROUP_A, HEAD_DIM_A)
    pad = ((0, 0), (BLOCK, BLOCK), (0, 0), (0, 0))
    kp = jnp.pad(k, pad).reshape(b, nb + 2, BLOCK, N_KV_A, HEAD_DIM_A)
    vp = jnp.pad(v, pad).reshape(b, nb + 2, BLOCK, N_KV_A, HEAD_DIM_A)
    kw = jnp.concatenate([kp[:, :-2], kp[:, 1:-1], kp[:, 2:]], axis=2)
    vw = jnp.concatenate([vp[:, :-2], vp[:, 1:-1], vp[:, 2:]], axis=2)
    scores = jnp.einsum('bnqhgd,bnkhd->bnhgqk', qb, kw).astype(jnp.float32) * (HEAD_DIM_A ** -0.5)
    blk = jnp.arange(nb, dtype=jnp.int32)[:, None] * BLOCK
    qpos = blk + jnp.arange(BLOCK, dtype=jnp.int32)[None, :]
    kpos = blk - BLOCK + jnp.arange(3 * BLOCK, dtype=jnp.int32)[None, :]
    dist = qpos[:, :, None] - kpos[:, None, :]
    valid = (jnp.abs(dist) <= WINDOW) & (kpos[:, None, :] >= 0) & (kpos[:, None, :] < s)
    scores = jnp.where(valid[None, :, None, None, :, :], scores, NEG_INF)
    sink_col = jnp.broadcast_to(sink.astype(jnp.float32).reshape(1, 1, N_KV_A, GROUP_A, 1, 1),
                                scores.shape[:-1] + (1,))
    probs = jax.nn.softmax(jnp.concatenate([scores, sink_col], axis=-1), axis=-1)[..., :-1]
    out = jnp.einsum('bnhgqk,bnkhd->bnqhgd', probs.astype(v.dtype), vw)
    return out.reshape(b, s, A_Q)


def multiscale_pool(u, w_pool, scale):
    b, s, _ = u.shape
    uf = u.astype(jnp.float32).reshape(b, s, N_POOL_GROUPS, POOL_GROUP).transpose(0, 2, 1, 3)
    csum = jnp.pad(jnp.cumsum(uf, axis=2), ((0, 0), (0, 0), (1, 0), (0, 0)))
    half = jnp.array(POOL_WINDOWS, dtype=jnp.int32)[:, None] // 2
    t = jnp.arange(s, dtype=jnp.int32)[None, :]
    lo = jnp.clip(t - half, 0, s)
    hi = jnp.clip(t + half, 0, s)
    gi = jnp.arange(N_POOL_GROUPS, dtype=jnp.int32)[:, None]
    win_sum = csum[:, gi, hi, :] - csum[:, gi, lo, :]
    count = (hi - lo).astype(jnp.float32)[None, :, :, None]
    mixed = win_sum / count - uf
    y = jnp.einsum('bgsc,gcd->bsgd', mixed, w_pool.astype(jnp.float32)).reshape(b, s, POOL_WIDTH)
    return (y * scale.astype(jnp.float32)).astype(u.dtype)


def dense_block_attention(q, k, v):
    b, s, _, _ = q.shape
    nb = s // BLOCK
    qb = q.reshape(b, nb, BLOCK, N_KV_C, GROUP_C, HEAD_DIM_C).transpose(1, 0, 2, 3, 4, 5)

    def one_block(qblk):
        sc = jnp.einsum('bqhgd,bkhd->bhgqk', qblk, k).astype(jnp.float32) * (HEAD_DIM_C ** -0.5)
        p = jax.nn.softmax(sc, axis=-1)
        return jnp.einsum('bhgqk,bkhd->bqhgd', p.astype(v.dtype), v)

    out = lax.map(one_block, qb)
    return out.transpose(1, 0, 2, 3, 4, 5).reshape(b, s, C_Q)


def even_mixer(h, w_in, w_out, sink, w_pool, pool_scale, cos_a, sin_a):
    b, s, _ = h.shape
    proj = h @ w_in
    q, k, v, u = jnp.split(proj, [A_Q, A_Q + A_KV, A_Q + 2 * A_KV], axis=-1)
    q = apply_rope(q.reshape(b, s, N_HEADS_A, HEAD_DIM_A), cos_a, sin_a)
    k = apply_rope(k.reshape(b, s, N_KV_A, HEAD_DIM_A), cos_a, sin_a)
    v = v.reshape(b, s, N_KV_A, HEAD_DIM_A)
    attn = windowed_sink_attention(q, k, v, sink)
    pool = multiscale_pool(u, w_pool, pool_scale)
    return jnp.concatenate([attn, pool], axis=-1) @ w_out


def odd_mixer(h, w_qkv, q_gain, k_gain, w_out, cos_r, sin_r, cos_c, sin_c):
    b, s, _ = h.shape
    proj = h @ w_qkv
    q, k, v = jnp.split(proj, [C_Q, C_Q + C_KV], axis=-1)
    q = rms_norm(q.reshape(b, s, N_HEADS_C, HEAD_DIM_C), q_gain)
    k = rms_norm(k.reshape(b, s, N_KV_C, HEAD_DIM_C), k_gain)
    q = apply_axial_rope(q, cos_r, sin_r, cos_c, sin_c)
    k = apply_axial_rope(k, cos_r, sin_r, cos_c, sin_c)
    v = v.reshape(b, s, N_KV_C, HEAD_DIM_C)
    return dense_block_attention(q, k, v) @ w_out


def expert_choice_ffn(h, w_router, w_gate, w_up, w_down):
    b, s, d = h.shape
    cap = CAPACITY_FACTOR * s // N_EXPERTS
    aff = jax.nn.softmax(jnp.einsum('bsd,de->bse', h, w_router).astype(jnp.float32), axis=-1)
    gate, idx = lax.top_k(aff.transpose(0, 2, 1), cap)
    xs = jax.vmap(lambda hb, ib: hb[ib])(h, idx)
    g = jnp.einsum('becd,edf->becf', xs, w_gate)
    u = jnp.einsum('becd,edf->becf', xs, w_up)
    y = jnp.einsum('becf,efd->becd', jax.nn.silu(g) * u, w_down) * gate[..., None].astype(h.dtype)
    out = jax.vmap(lambda ib, yb: jnp.zeros((s, d), yb.dtype).at[ib.reshape(-1)].add(yb.reshape(-1, d)))(idx, y)
    return out


def setup_inputs(seed: int = 0) -> dict:
    key = jax.random.key(seed)
    ks = jax.random.split(key, 20)
    f32 = jnp.float32
    nrm = lambda k, shape, scale: jax.random.normal(k, shape, f32) * scale
    return {
        "x": nrm(ks[0], (BATCH, SEQ, D_MODEL), 1.0),
        "norm_mix": 1.0 + nrm(ks[1], (DEPTH, D_MODEL), 0.02),
        "norm_ffn": 1.0 + nrm(ks[2], (DEPTH, D_MODEL), 0.02),
        "norm_final": 1.0 + nrm(ks[3], (D_MODEL,), 0.02),
        "a_w_in": nrm(ks[4], (N_EVEN, D_MODEL, EVEN_IN), D_MODEL ** -0.5),
        "a_w_out": nrm(ks[5], (N_EVEN, EVEN_OUT, D_MODEL), EVEN_OUT ** -0.5),
        "a_sink": nrm(ks[6], (N_EVEN, N_HEADS_A), 0.5),
        "b_w_pool": nrm(ks[7], (N_EVEN, N_POOL_GROUPS, POOL_GROUP, POOL_GROUP), POOL_GROUP ** -0.5),
        "b_scale": 1.0 + nrm(ks[8], (N_EVEN, POOL_WIDTH), 0.02),
        "c_w_qkv": nrm(ks[9], (N_ODD, D_MODEL, ODD_IN), D_MODEL ** -0.5),
        "c_q_norm": 1.0 + nrm(ks[10], (N_ODD, HEAD_DIM_C), 0.02),
        "c_k_norm": 1.0 + nrm(ks[11], (N_ODD, HEAD_DIM_C), 0.02),
        "c_w_out": nrm(ks[12], (N_ODD, C_Q, D_MODEL), C_Q ** -0.5),
        "moe_router": nrm(ks[13], (DEPTH, D_MODEL, N_EXPERTS), D_MODEL ** -0.5),
        "moe_w_gate": nrm(ks[14], (DEPTH, N_EXPERTS, D_MODEL, EXPERT_FF), D_MODEL ** -0.5),
        "moe_w_up": nrm(ks[15], (DEPTH, N_EXPERTS, D_MODEL, EXPERT_FF), D_MODEL ** -0.5),
        "moe_w_down": nrm(ks[16], (DEPTH, N_EXPERTS, EXPERT_FF, D_MODEL), EXPERT_FF ** -0.5),
    }


def reference(x, norm_mix, norm_ffn, norm_final, a_w_in, a_w_out, a_sink, b_w_pool, b_scale,
              c_w_qkv, c_q_norm, c_k_norm, c_w_out, moe_router, moe_w_gate, moe_w_up, moe_w_down):
    s = x.shape[1]
    rows = s // GRID_W
    t = jnp.arange(s, dtype=jnp.int32)
    cos_a, sin_a = rope_cos_sin(t, HEAD_DIM_A)
    row_idx = jnp.repeat(jnp.arange(rows, dtype=jnp.int32), GRID_W)
    col_idx = jnp.tile(jnp.arange(GRID_W, dtype=jnp.int32), rows)
    cos_r, sin_r = rope_cos_sin(row_idx, AXIAL_DIM)
    cos_c, sin_c = rope_cos_sin(col_idx, AXIAL_DIM)
    for i in range(DEPTH):
        j = i // 2
        h = rms_norm(x, norm_mix[i])
        if i % 2 == 0:
            x = x + even_mixer(h, a_w_in[j], a_w_out[j], a_sink[j], b_w_pool[j], b_scale[j], cos_a, sin_a)
        else:
            x = x + odd_mixer(h, c_w_qkv[j], c_q_norm[j], c_k_norm[j], c_w_out[j], cos_r, sin_r, cos_c, sin_c)
        h = rms_norm(x, norm_ffn[i])
        x = x + expert_choice_ffn(h, moe_router[i], moe_w_gate[i], moe_w_up[i], moe_w_down[i])
    return rms_norm(x, norm_final)
```

```python
import numpy as np
import ml_dtypes
import concourse.bass as bass
import concourse.mybir as mybir
from concourse.bass_utils import run_bass_kernel_spmd
from contextlib import ExitStack

F32 = mybir.dt.float32
BF16 = mybir.dt.bfloat16
I32 = mybir.dt.int32
AF = mybir.ActivationFunctionType
ALU = mybir.AluOpType
AX = mybir.AxisListType
BF = ml_dtypes.bfloat16
SEQ = 8192
POOL_WINDOWS = (2, 4, 8, 16)


class T:
    def __init__(self, name, ap):
        self.name = name
        self.ap = ap
        self.w = None
        self.r = []
    def __getitem__(self, k):
        return self.ap[k]


class Chan:
    def __init__(self, name, sem, step):
        self.name, self.sem, self.step, self.val = name, sem, step, 0


class MK:
    ENGS = ["tensor", "vector", "scalar", "gpsimd", "sync"]

    def __init__(self, nc, es):
        self.nc = nc
        self.es = es
        self.ses = None
        self.q = {e: [] for e in self.ENGS}
        self.prog = {}
        for e in self.ENGS:
            s = es.enter_context(nc.semaphore("p_" + e))
            self.prog[e] = Chan(e, s, 1)
        self.waited = {e: {} for e in self.ENGS}
        self.pool = []
        self.pool_i = 0
        self.uid = 0

    def begin_stage(self):
        self.ses = ExitStack()
        self.pool_i = 0
        chans = list(self.prog.values()) + self.pool
        for eng in self.ENGS:
            for ch in chans:
                if ch.val > 0 and self.waited[eng].get(ch.name, -1) < ch.val:
                    self.waited[eng][ch.name] = ch.val
                    self.q[eng].append(lambda e, s=ch.sem, v=ch.val: e.wait_ge(s, v))

    def end_stage(self):
        self.flush()
        self.ses.close()
        self.ses = None

    def flush(self):
        nc = self.nc
        q = self.q
        with nc.Block() as block:
            @block.tensor
            def _(e):
                for f in q["tensor"]: f(e)
            @block.vector
            def _(e):
                for f in q["vector"]: f(e)
            @block.scalar
            def _(e):
                for f in q["scalar"]: f(e)
            @block.gpsimd
            def _(e):
                for f in q["gpsimd"]: f(e)
            @block.sync
            def _(e):
                for f in q["sync"]: f(e)
        self.q = {e: [] for e in self.ENGS}

    def chan(self, name=None):
        if self.pool_i < len(self.pool):
            ch = self.pool[self.pool_i]
        else:
            s = self.es.enter_context(self.nc.semaphore("c_%d" % len(self.pool)))
            ch = Chan("c_%d" % len(self.pool), s, 16)
            self.pool.append(ch)
        self.pool_i += 1
        return ch

    def sb(self, name, shape, dt, glob=False):
        self.uid += 1
        st = self.es if (glob or self.ses is None) else self.ses
        t = st.enter_context(self.nc.sbuf_tensor("%s_%d" % (name, self.uid), list(shape), dt))
        return T(name, t)

    def ps(self, name, shape, dt=F32):
        t = self.es.enter_context(self.nc.psum_tensor(name, list(shape), dt))
        return T(name, t)

    def _deps(self, eng, reads, writes):
        deps = {}
        def add(tok):
            if tok is None:
                return
            ch, v = tok
            if ch.name == eng and eng == "tensor":
                return
            if deps.get(ch.name, (None, -1))[1] < v:
                deps[ch.name] = (ch, v)
        for t in reads:
            add(t.w)
        for t in writes:
            add(t.w)
            for r in t.r:
                add(r)
        out = []
        for name, (ch, v) in deps.items():
            if self.waited[eng].get(name, -1) >= v:
                continue
            self.waited[eng][name] = v
            out.append((ch.sem, v))
        return out

    def _record(self, tok, reads, writes):
        for t in reads:
            t.r.append(tok)
            if len(t.r) > 64:
                best = {}
                for (c, v) in t.r:
                    if best.get(c.name, (None, -1))[1] < v:
                        best[c.name] = (c, v)
                t.r = list(best.values())
        for t in writes:
            t.w = tok
            t.r = []

    def op(self, eng, fn, reads=(), writes=()):
        waits = self._deps(eng, reads, writes)
        ch = self.prog[eng]
        ch.val += 1
        tok = (ch, ch.val)
        sem = ch.sem
        def run(e, waits=waits, fn=fn, sem=sem):
            for (s, v) in waits:
                e.wait_ge(s, v)
            fn(e).then_inc(sem, 1)
        self.q[eng].append(run)
        self._record(tok, reads, writes)
        return tok

    def dma(self, eng, ch, fn, reads=(), writes=()):
        waits = self._deps(eng, reads, writes)
        ch.val += 16
        tok = (ch, ch.val)
        sem = ch.sem
        def run(e, waits=waits, fn=fn, sem=sem):
            for (s, v) in waits:
                e.wait_ge(s, v)
            fn(e).then_inc(sem, 16)
        self.q[eng].append(run)
        self._record(tok, reads, writes)
        return tok

    def seal(self, ch, tiles):
        for t in tiles:
            t.w = (ch, ch.val)

    def wait_tok(self, eng, tok):
        ch, v = tok
        if self.waited[eng].get(ch.name, -1) >= v:
            return
        self.waited[eng][ch.name] = v
        self.q[eng].append(lambda e, s=ch.sem, v=v: e.wait_ge(s, v))


def stage_A(mk, i, D, G):
    even = (i % 2 == 0)
    j = i // 2
    N = 1280 if even else 1536
    NT = 64
    x_src = D["x"] if i == 0 else D["xbuf"]
    w = D["a_w_in"][j] if even else D["c_w_qkv"][j]
    cs = D["cs_even"] if even else D["cs_odd"]
    HD = 64 if even else 128
    QT = D["QT_e"] if even else D["QT_o"]
    KT = D["KT_e"] if even else D["KT_o"]
    VA = D["VA_e"] if even else D["VA_o"]
    mk.begin_stage()
    cw = mk.chan(); cc = mk.chan()
    ldx = [mk.chan() for _ in range(2)]
    ldc = [mk.chan() for _ in range(2)]
    sto = [mk.chan() for _ in range(2)]
    idb = G["idb"]
    TP = G["TP"]; pp = G["PS"][0:3]
    wbf = mk.sb("wbf", [128, 8, N], BF16)
    gt = mk.sb("gt", [128, 1024], F32)
    for kc in range(8):
        mk.dma("gpsimd", cw, lambda e, kc=kc: e.dma_start(out=wbf[:, kc, :], in_=w[kc*128:(kc+1)*128, :]), writes=[wbf])
    mk.dma("sync", cc, lambda e: e.dma_start(out=gt[:], in_=D["norm_mix"][i:i+1, :].partition_broadcast(128)), writes=[gt])
    if not even:
        gn = mk.sb("gn", [128, 1280], F32)
        mk.dma("sync", cc, lambda e: e.dma_start(out=gn[:], in_=D["gain"][j:j+1, :].partition_broadcast(128)), writes=[gn])
    mk.seal(cc, [gt] + ([] if even else [gn]))
    xt = [mk.sb(f"xt{k}", [128, 1024], F32) for k in range(2)]
    sq = mk.sb("sq", [128, 1024], F32)
    st = [mk.sb(f"st{k}", [128, 4], F32) for k in range(2)]
    hb = [mk.sb(f"hb{k}", [128, 1024], BF16) for k in range(2)]
    hT = [mk.sb(f"hT{k}", [128, 8, 128], BF16) for k in range(2)]
    pf = [mk.sb(f"pf{k}", [128, N], F32) for k in range(2)]
    ot = [mk.sb(f"ot{k}", [128, N], BF16) for k in range(2)]
    cst = [mk.sb(f"cst{k}", [128, 2, 32 if even else 64], F32) for k in range(2)]
    tmp = [mk.sb(f"tmp{k}", [128, 4, 640], F32) for k in range(2)]
    tq = [mk.sb(f"tq{k}", [128, 10, 128], BF16) for k in range(2)]
    VW = HD + 1 if even else HD
    vaug = [mk.sb(f"vaug{k}", [128, 2, VW], BF16) for k in range(2)]
    for k in range(2):
        mk.op("vector", lambda e, k=k: e.memset(vaug[k][:], 1.0), writes=[vaug[k]])
    if not even:
        ms = [mk.sb(f"ms{k}", [128, 16], F32) for k in range(2)]
    def front(t):
        b = t % 2
        r0 = t * 128
        mk.dma("sync", ldx[b], lambda e, b=b, r0=r0: e.dma_start(out=xt[b][:], in_=x_src[r0:r0+128, :]), writes=[xt[b]])
        mk.dma("sync", ldc[b], lambda e, b=b, r0=r0: e.dma_start(out=cst[b][:], in_=cs[r0:r0+128]), writes=[cst[b]])
        mk.op("scalar", lambda e, b=b: e.activation(out=sq[:], in_=xt[b][:], func=AF.Square, accum_out=st[b][:, 0:1]), reads=[xt[b]], writes=[sq, st[b]])
        mk.op("scalar", lambda e, b=b: e.activation(out=st[b][:, 1:2], in_=st[b][:, 0:1], func=AF.Sqrt, scale=1.0/1024, bias=1e-6), reads=[st[b]], writes=[st[b]])
        mk.op("vector", lambda e, b=b: e.reciprocal(out=st[b][:, 2:3], in_=st[b][:, 1:2]), reads=[st[b]], writes=[st[b]])
        mk.op("vector", lambda e, b=b: e.scalar_tensor_tensor(out=hb[b][:], in0=xt[b][:], scalar=st[b][:, 2:3], in1=gt[:], op0=ALU.mult, op1=ALU.mult), reads=[xt[b], st[b], gt], writes=[hb[b]])
        for kc in range(8):
            mk.op("tensor", lambda e, b=b, kc=kc: e.transpose(out=TP[:, kc, :], in_=hb[b][:, kc*128:(kc+1)*128], identity=idb[:]), reads=[hb[b], idb], writes=[TP])
        mk.op("scalar", lambda e, b=b: e.copy(out=hT[b][:], in_=TP[:]), reads=[TP], writes=[hT[b]])
        nblocks = [(n0, min(512, N - n0)) for n0 in range(0, N, 512)]
        for bi, (n0, nw) in enumerate(nblocks):
            for kc in range(8):
                mk.op("tensor", lambda e, b=b, kc=kc, bi=bi, n0=n0, nw=nw: e.matmul(pp[bi][:, 0:nw], lhsT=hT[b][:, kc, :], rhs=wbf[:, kc, n0:n0+nw], start=(kc == 0), stop=(kc == 7)), reads=[hT[b], wbf], writes=[pp[bi]])
            mk.op("scalar", lambda e, b=b, bi=bi, n0=n0, nw=nw: e.copy(out=pf[b][:, n0:n0+nw], in_=pp[bi][:, 0:nw]), reads=[pp[bi]], writes=[pf[b]])

    def back(t):
        b = t % 2
        r0 = t * 128
        NH = 10
        if even:
            v4 = pf[b][:, 0:640].rearrange("p (h two j) -> p h two j", two=2, j=32)
            o4 = ot[b][:, 0:640].rearrange("p (h two j) -> p h two j", two=2, j=32)
            x1, x2 = v4[:, :, 0, :], v4[:, :, 1, :]
            o1, o2 = o4[:, :, 0, :], o4[:, :, 1, :]
            cosb = cst[b][:, 0, :].unsqueeze(1).broadcast_to([128, NH, 32])
            sinb = cst[b][:, 1, :].unsqueeze(1).broadcast_to([128, NH, 32])
            tm = [tmp[b][:, k, 0:NH*32].rearrange("p (h j) -> p h j", j=32) for k in range(4)]
        else:
            v3 = pf[b][:, 0:1280].rearrange("p (h d) -> p h d", d=128)
            mk.op("vector", lambda e, b=b: e.tensor_tensor(out=sq[:, 0:1024], in0=pf[b][:, 0:1024], in1=pf[b][:, 0:1024], op=ALU.mult), reads=[pf[b]], writes=[sq])
            mk.op("vector", lambda e, b=b: e.tensor_reduce(out=ms[b][:, 0:8], in_=sq[:, 0:1024].rearrange("p (h d) -> p h d", d=128), axis=AX.X, op=ALU.add), reads=[sq], writes=[ms[b]])
            mk.op("vector", lambda e, b=b: e.tensor_tensor(out=sq[:, 0:256], in0=pf[b][:, 1024:1280], in1=pf[b][:, 1024:1280], op=ALU.mult), reads=[pf[b]], writes=[sq])
            mk.op("vector", lambda e, b=b: e.tensor_reduce(out=ms[b][:, 8:10], in_=sq[:, 0:256].rearrange("p (h d) -> p h d", d=128), axis=AX.X, op=ALU.add), reads=[sq], writes=[ms[b]])
            mk.op("scalar", lambda e, b=b: e.activation(out=ms[b][:, 0:10], in_=ms[b][:, 0:10], func=AF.Sqrt, scale=1.0/128, bias=1e-6), reads=[ms[b]], writes=[ms[b]])
            mk.op("vector", lambda e, b=b: e.reciprocal(out=ms[b][:, 0:10], in_=ms[b][:, 0:10]), reads=[ms[b]], writes=[ms[b]])
            mk.op("vector", lambda e, b=b, v3=v3: e.tensor_tensor(out=v3, in0=v3, in1=ms[b][:, 0:10].unsqueeze(2).broadcast_to([128, 10, 128]), op=ALU.mult), reads=[pf[b], ms[b]], writes=[pf[b]])
            mk.op("vector", lambda e, b=b: e.tensor_tensor(out=pf[b][:, 0:1280], in0=pf[b][:, 0:1280], in1=gn[:], op=ALU.mult), reads=[pf[b], gn], writes=[pf[b]])
            v5 = pf[b][:, 0:1280].rearrange("p (h a two j) -> p h a two j", a=2, two=2, j=32)
            o5 = ot[b][:, 0:1280].rearrange("p (h a two j) -> p h a two j", a=2, two=2, j=32)
            x1, x2 = v5[:, :, :, 0, :], v5[:, :, :, 1, :]
            o1, o2 = o5[:, :, :, 0, :], o5[:, :, :, 1, :]
            cosb = cst[b][:, 0, :].rearrange("p (a j) -> p a j", j=32).unsqueeze(1).broadcast_to([128, NH, 2, 32])
            sinb = cst[b][:, 1, :].rearrange("p (a j) -> p a j", j=32).unsqueeze(1).broadcast_to([128, NH, 2, 32])
            tm = [tmp[b][:, k, :].rearrange("p (h a j) -> p h a j", a=2, j=32) for k in range(4)]
        R = [pf[b], cst[b]]
        mk.op("vector", lambda e, x1=x1, cosb=cosb, tm=tm: e.tensor_tensor(out=tm[0], in0=x1, in1=cosb, op=ALU.mult), reads=R, writes=[tmp[b]])
        mk.op("gpsimd", lambda e, x2=x2, sinb=sinb, tm=tm: e.tensor_tensor(out=tm[1], in0=x2, in1=sinb, op=ALU.mult), reads=R, writes=[tmp[b]])
        mk.op("vector", lambda e, x1=x1, sinb=sinb, tm=tm: e.tensor_tensor(out=tm[2], in0=x1, in1=sinb, op=ALU.mult), reads=R, writes=[tmp[b]])
        mk.op("gpsimd", lambda e, x2=x2, cosb=cosb, tm=tm: e.tensor_tensor(out=tm[3], in0=x2, in1=cosb, op=ALU.mult), reads=R, writes=[tmp[b]])
        mk.op("vector", lambda e, o1=o1, tm=tm: e.tensor_tensor(out=o1, in0=tm[0], in1=tm[1], op=ALU.subtract), reads=[tmp[b]], writes=[ot[b]])
        mk.op("vector", lambda e, o2=o2, tm=tm: e.tensor_tensor(out=o2, in0=tm[2], in1=tm[3], op=ALU.add), reads=[tmp[b]], writes=[ot[b]])
        if even:
            mk.op("scalar", lambda e, b=b: e.copy(out=ot[b][:, 768:1280], in_=pf[b][:, 768:1280]), reads=[pf[b]], writes=[ot[b]])
            mk.op("scalar", lambda e, b=b: e.copy(out=vaug[b][:, :, 0:64], in_=pf[b][:, 640:768].rearrange("p (g d) -> p g d", d=64)), reads=[pf[b]], writes=[vaug[b]])
            for c in range(5):
                mk.op("tensor", lambda e, b=b, c=c: e.transpose(out=TP[:, c, :], in_=ot[b][:, c*128:(c+1)*128], identity=idb[:]), reads=[ot[b], idb], writes=[TP])
            mk.op("scalar", lambda e, b=b: e.copy(out=tq[b][:, 0:5, :], in_=TP[:, 0:5, :]), reads=[TP], writes=[tq[b]])
            for pr in range(2):
                for g in range(2):
                    dst = QT[g, t].rearrange("d (cc two qq) -> d cc two qq", two=2, qq=128)[:, :, pr, :]
                    mk.dma("sync", sto[b], lambda e, b=b, pr=pr, g=g, dst=dst: e.dma_start(out=dst, in_=tq[b][pr*64:(pr+1)*64, g*2:(g+1)*2, :]), reads=[tq[b]])
            for g in range(2):
                mk.dma("gpsimd", sto[b], lambda e, b=b, g=g, r0=r0: e.dma_start(out=KT[g, :, r0:r0+128], in_=tq[b][g*64:(g+1)*64, 4, :]), reads=[tq[b]])
            mk.dma("gpsimd", sto[b], lambda e, b=b, r0=r0: e.dma_start(out=D["Ubuf"][r0:r0+128, :], in_=ot[b][:, 768:1280]), reads=[ot[b]])
        else:
            mk.op("scalar", lambda e, b=b: e.copy(out=vaug[b][:, :, 0:128], in_=pf[b][:, 1280:1536].rearrange("p (g d) -> p g d", d=128)), reads=[pf[b]], writes=[vaug[b]])
            for h in range(8):
                mk.op("tensor", lambda e, b=b, h=h: e.transpose(out=TP[:, h, :], in_=ot[b][:, h*128:(h+1)*128], identity=idb[:]), reads=[ot[b], idb], writes=[TP])
            mk.op("scalar", lambda e, b=b: e.copy(out=tq[b][:, 0:8, :], in_=TP[:]), reads=[TP], writes=[tq[b]])
            for g in range(2):
                mk.op("tensor", lambda e, b=b, g=g: e.transpose(out=TP[:, g, :], in_=ot[b][:, 1024+g*128:1024+(g+1)*128], identity=idb[:]), reads=[ot[b], idb], writes=[TP])
            mk.op("scalar", lambda e, b=b: e.copy(out=tq[b][:, 8:10, :], in_=TP[:, 0:2, :]), reads=[TP], writes=[tq[b]])
            for g in range(2):
                mk.dma("sync", sto[b], lambda e, b=b, g=g, t=t: e.dma_start(out=QT[g, t].rearrange("d (hh qq) -> d hh qq", qq=128), in_=tq[b][:, g*4:(g+1)*4, :]), reads=[tq[b]])
                mk.dma("gpsimd", sto[b], lambda e, b=b, g=g, r0=r0: e.dma_start(out=KT[g, :, r0:r0+128], in_=tq[b][:, 8+g, :]), reads=[tq[b]])
        mk.dma("gpsimd", sto[b], lambda e, b=b, t=t: e.dma_start(out=VA[:, t].rearrange("g kp c -> kp g c"), in_=vaug[b][:]), reads=[vaug[b]])

    front(0)
    for t in range(NT):
        if t + 1 < NT:
            front(t + 1)
        back(t)
    mk.end_stage()


def stage_B(mk, i, D, G):
    even = (i % 2 == 0)
    j = i // 2
    HD = 64 if even else 128
    NKT = 64
    x_src = D["x"] if i == 0 else D["xbuf"]
    QT = D["QT_e"] if even else D["QT_o"]
    KT = D["KT_e"] if even else D["KT_o"]
    VA = D["VA_e"] if even else D["VA_o"]
    w_out = D["a_w_out"][j] if even else D["c_w_out"][j]
    scale = HD ** -0.5
    mk.begin_stage()
    cc = mk.chan(); cw = mk.chan()
    ck = [mk.chan() for _ in range(2)]
    cq = [mk.chan() for _ in range(2)]
    cx = [mk.chan() for _ in range(2)]
    cu = [mk.chan() for _ in range(2)]
    so = [mk.chan() for _ in range(2)]
    idb = G["idb"]; idf = G["idf"]; aff_sb = G["aff_sb"]
    PS = G["PS"]; TP = G["TP"]
    S = PS[0:2]; O = PS[2:6]; X = PS[6]
    wo = mk.sb("wo", [128, 8, 1024], BF16)
    for kc in range(8):
        mk.dma("gpsimd", cw, lambda e, kc=kc: e.dma_start(out=wo[:, kc, :], in_=w_out[kc*128:(kc+1)*128, :]), writes=[wo])
    gt = mk.sb("gt", [128, 1024], F32)
    mk.dma("sync", cc, lambda e: e.dma_start(out=gt[:], in_=D["norm_ffn"][i:i+1, :].partition_broadcast(128)), writes=[gt])
    wr = mk.sb("wr", [128, 8, 16], F32)
    mk.dma("sync", cc, lambda e: e.dma_start(out=wr[:], in_=D["moe_router"][i].rearrange("(kc p) e -> p kc e", p=128)), writes=[wr])
    consts = [gt, wr]
    if even:
        msk = mk.sb("msk", [128, 2, 128], BF16)
        mk.dma("sync", cc, lambda e: e.dma_start(out=msk[:], in_=D["masks"][:]), writes=[msk])
        snk = mk.sb("snk", [128, 8], F32)
        mk.dma("sync", cc, lambda e: e.dma_start(out=snk[:], in_=D["a_sink"][j:j+1, :].partition_broadcast(128)), writes=[snk])
        bm = mk.sb("bm", [128, 3, 4, 128], BF16)
        bh = mk.sb("bh", [16, 4, 128], BF16)
        mk.dma("sync", cc, lambda e: e.dma_start(out=bm[:], in_=D["Bm"][:]), writes=[bm])
        mk.dma("sync", cc, lambda e: e.dma_start(out=bh[:], in_=D["Bh"][:]), writes=[bh])
        bs = mk.sb("bs", [128, 4], F32)
        mk.dma("sync", cc, lambda e: e.dma_start(out=bs[:], in_=D["bsc"][j]), writes=[bs])
        consts += [msk, snk, bm, bh, bs]
    mk.seal(cc, consts)
    if even:
        mk.op("scalar", lambda e: e.activation(out=snk[:], in_=snk[:], func=AF.Exp), reads=[snk], writes=[snk])
        wp = mk.sb("wp", [128, 4, 128], BF16)
        for gi in range(4):
            mk.dma("gpsimd", cw, lambda e, gi=gi: e.dma_start(out=wp[:, gi, :], in_=D["b_w_pool"][j, gi]), writes=[wp])
    attnT = mk.sb("attnT", [128, 8, 2048], BF16)
    ksb = [mk.sb(f"ksb{k}", [128, 8192], BF16) for k in range(2)]
    VW = HD + 1 if even else HD
    vsb = [mk.sb(f"vsb{k}", [128, NKT, VW], BF16) for k in range(2)]
    qsb = [mk.sb(f"qsb{k}", [128, 512], BF16) for k in range(2)]
    if even:
        for k in range(2):
            mk.op("gpsimd", lambda e, k=k: e.memset(ksb[k][:], 0.0), writes=[ksb[k]])
            mk.op("vector", lambda e, k=k: e.memset(qsb[k][:], 0.0), writes=[qsb[k]])
    pT = [mk.sb(f"pT{k}", [128, 512], BF16) for k in range(4)]
    asb = [mk.sb(f"asb{k}", [128, 512], BF16) for k in range(2)]
    rc = [mk.sb(f"rc{k}", [128, 4], F32) for k in range(2)]
    if not even:
        accA = mk.sb("accA", [128, 512], F32)
        accB = mk.sb("accB", [128, 512], F32)
        rinv = mk.sb("rinv", [128, 512], F32)
    for g in range(2):
        mk.dma("sync", ck[g], lambda e, g=g: e.dma_start(out=ksb[g][0:HD, :], in_=KT[g]), writes=[ksb[g]])
        for hf in range(4):
            mk.dma("sync", ck[g], lambda e, g=g, hf=hf: e.dma_start(out=vsb[g][:, hf*16:(hf+1)*16, :], in_=VA[g, hf*16:(hf+1)*16].rearrange("kt kp c -> kp kt c")), writes=[vsb[g]])
        mk.seal(ck[g], [ksb[g], vsb[g]])
    xt = [mk.sb(f"xt{k}", [128, 1024], F32) for k in range(2)]
    sq = mk.sb("sq", [128, 1024], F32)
    st = [mk.sb(f"st{k}", [128, 4], F32) for k in range(2)]
    h2f = [mk.sb(f"h2f{k}", [128, 1024], F32) for k in range(2)]
    h2b = [mk.sb(f"h2b{k}", [128, 1024], BF16) for k in range(2)]
    h2T = [mk.sb(f"h2T{k}", [128, 8, 128], F32) for k in range(2)]
    ex = [mk.sb(f"ex{k}", [128, 16], F32) for k in range(2)]
    if even:
        um = [mk.sb(f"um{k}", [128, 512], BF16) for k in range(2)]
        uh = [mk.sb(f"uh{k}", [16, 512], BF16) for k in range(2)]
        mT = [mk.sb(f"mT{k}", [128, 4, 128], BF16) for k in range(2)]
    qi = 0
    for chk in range(4):
        for g in range(2):
            for ql in range(16):
                qt = chk * 16 + ql
                qb = qi % 2; qi += 1
                mk.dma("sync", cq[qb], lambda e, qb=qb, g=g, qt=qt: e.dma_start(out=qsb[qb][0:HD, :], in_=QT[g, qt]), writes=[qsb[qb]])
                if not even:
                    nk = 64
                    LA = 2
                    OT = O[0]; RS = O[1]
                    S4 = [PS[0], PS[1], PS[4], PS[5]]
                    for ii in range(nk + LA):
                        if ii < nk:
                            kt = ii
                            sb_ = S4[ii % 4]; pb = pT[ii % 4]
                            mk.op("tensor", lambda e, sb_=sb_, g=g, kt=kt, qb=qb: e.matmul(sb_[:], lhsT=ksb[g][:, kt*128:(kt+1)*128], rhs=qsb[qb][:], start=True, stop=True), reads=[ksb[g], qsb[qb]], writes=[sb_])
                            mk.op("scalar", lambda e, sb_=sb_, pb=pb: e.activation(out=pb[:], in_=sb_[:], func=AF.Exp, scale=scale), reads=[sb_], writes=[pb])
                            if kt % 3 != 2:
                                if kt == 0:
                                    mk.op("vector", lambda e, pb=pb: e.tensor_copy(out=accA[:], in_=pb[:]), reads=[pb], writes=[accA])
                                else:
                                    mk.op("vector", lambda e, pb=pb: e.tensor_tensor(out=accA[:], in0=accA[:], in1=pb[:], op=ALU.add), reads=[pb, accA], writes=[accA])
                        if ii >= LA:
                            i2 = ii - LA
                            pb = pT[i2 % 4]
                            mk.op("tensor", lambda e, pb=pb, g=g, i2=i2, nk=nk: e.matmul(OT[:], lhsT=vsb[g][:, i2, :], rhs=pb[:], start=(i2 == 0), stop=(i2 == nk - 1)), reads=[pb, vsb[g]], writes=[OT])
                            if i2 % 3 == 2:
                                mk.op("tensor", lambda e, pb=pb, i2=i2: e.matmul(RS[:], lhsT=G["onesb"][:], rhs=pb[:], start=(i2 == 2), stop=False), reads=[pb, G["onesb"]], writes=[RS])
                    mk.op("tensor", lambda e: e.matmul(RS[:], lhsT=G["cst"][:, 2, :], rhs=accA[:], start=False, stop=True), reads=[G["cst"], accA], writes=[RS])
                    mk.op("vector", lambda e: e.reciprocal(out=rinv[:], in_=RS[:]), reads=[RS], writes=[rinv])
                    mk.op("vector", lambda e, g=g, ql=ql: e.tensor_tensor(out=attnT[:, g*4:(g+1)*4, ql*128:(ql+1)*128], in0=OT[:].rearrange("p (h q) -> p h q", q=128), in1=rinv[:].rearrange("p (h q) -> p h q", q=128), op=ALU.mult), reads=[OT, rinv], writes=[attnT])
                    continue
                if even:
                    kts = [(kt, kt - qt) for kt in (qt - 1, qt, qt + 1) if 0 <= kt < 64]
                else:
                    kts = [(kt, 0) for kt in range(64)]
                nk = len(kts)
                for ii in range(nk + 1):
                    if ii < nk:
                        kt, rel = kts[ii]
                        sb_ = S[ii % 2]; pb = pT[ii % 3]
                        mk.op("tensor", lambda e, sb_=sb_, g=g, kt=kt, qb=qb: e.matmul(sb_[:], lhsT=ksb[g][:, kt*128:(kt+1)*128], rhs=qsb[qb][:], start=True, stop=True), reads=[ksb[g], qsb[qb]], writes=[sb_])
                        mk.op("scalar", lambda e, sb_=sb_, pb=pb: e.activation(out=pb[:], in_=sb_[:], func=AF.Exp, scale=scale), reads=[sb_], writes=[pb])
                        if even and rel != 0:
                            mi = 0 if rel < 0 else 1
                            mk.op("vector", lambda e, pb=pb, mi=mi: e.tensor_tensor(out=pb[:].rearrange("p (h q) -> p h q", q=128), in0=pb[:].rearrange("p (h q) -> p h q", q=128), in1=msk[:, mi, :].unsqueeze(1).broadcast_to([128, 4, 128]), op=ALU.mult), reads=[pb, msk], writes=[pb])
                    if ii >= 1:
                        i2 = ii - 1
                        kt, rel = kts[i2]; pb = pT[i2 % 3]
                        for hh in range(4):
                            mk.op("tensor", lambda e, hh=hh, pb=pb, g=g, kt=kt, i2=i2, nk=nk: e.matmul(O[hh][:, 0:HD+1], lhsT=pb[:, hh*128:(hh+1)*128], rhs=vsb[g][:, kt, :], start=(i2 == 0), stop=(i2 == nk - 1)), reads=[pb, vsb[g]], writes=[O[hh]])
                ab = asb[qi % 2]; rb = rc[qi % 2]
                for hh in range(4):
                    if even:
                        mk.op("vector", lambda e, hh=hh, rb=rb, g=g: e.tensor_tensor(out=rb[:, hh:hh+1], in0=O[hh][:, HD:HD+1], in1=snk[:, g*4+hh:g*4+hh+1], op=ALU.add), reads=[O[hh], snk], writes=[rb])
                        mk.op("vector", lambda e, hh=hh, rb=rb: e.reciprocal(out=rb[:, hh:hh+1], in_=rb[:, hh:hh+1]), reads=[rb], writes=[rb])
                    else:
                        mk.op("vector", lambda e, hh=hh, rb=rb: e.reciprocal(out=rb[:, hh:hh+1], in_=O[hh][:, HD:HD+1]), reads=[O[hh]], writes=[rb])
                    mk.op("vector", lambda e, hh=hh, rb=rb, ab=ab: e.tensor_scalar(out=ab[:, hh*HD:(hh+1)*HD], in0=O[hh][:, 0:HD], scalar1=rb[:, hh:hh+1], scalar2=None, op0=ALU.mult), reads=[O[hh], rb], writes=[ab])
                nch = (4 * HD) // 128
                for c in range(nch):
                    mk.op("tensor", lambda e, c=c, ab=ab: e.transpose(out=TP[:, c, :], in_=ab[:, c*128:(c+1)*128], identity=idb[:]), reads=[ab, idb], writes=[TP])
                fc0 = g * nch
                mk.op("scalar", lambda e, fc0=fc0, nch=nch, ql=ql: e.copy(out=attnT[:, fc0:fc0+nch, ql*128:(ql+1)*128], in_=TP[:, 0:nch, :]), reads=[TP], writes=[attnT])
        if even:
            for tl in range(16):
                t = chk * 16 + tl
                b = t % 2
                var = 0 if t == 0 else (2 if t == 63 else 1)
                mk.dma("sync", cu[b], lambda e, b=b, t=t: e.dma_start(out=um[b][:], in_=D["Ubuf"][128*t: 128*t + 128, :]), writes=[um[b]])
                if t == 0 or t == 63:
                    mk.op("vector", lambda e, b=b: e.memset(uh[b][:], 0.0), writes=[uh[b]])
                if t > 0:
                    mk.dma("sync", cu[b], lambda e, b=b, t=t: e.dma_start(out=uh[b][0:8, :], in_=D["Ubuf"][128*t - 8: 128*t, :]), writes=[uh[b]])
                if t < 63:
                    mk.dma("sync", cu[b], lambda e, b=b, t=t: e.dma_start(out=uh[b][8:16, :], in_=D["Ubuf"][128*t + 128: 128*t + 136, :]), writes=[uh[b]])
                mk.seal(cu[b], [um[b], uh[b]])
                for gi in range(4):
                    mk.op("tensor", lambda e, b=b, gi=gi, var=var: e.matmul(S[0][:, gi*128:(gi+1)*128], lhsT=um[b][:, gi*128:(gi+1)*128], rhs=bm[:, var, gi, :], start=True, stop=False), reads=[um[b], bm], writes=[S[0]])
                    mk.op("tensor", lambda e, b=b, gi=gi: e.matmul(S[0][:, gi*128:(gi+1)*128], lhsT=uh[b][:, gi*128:(gi+1)*128], rhs=bh[:, gi, :], start=False, stop=True), reads=[uh[b], bh], writes=[S[0]])
                mk.op("vector", lambda e, b=b: e.tensor_copy(out=mT[b][:].rearrange("p g t -> p (g t)"), in_=S[0][:]), reads=[S[0]], writes=[mT[b]])
                for gi in range(4):
                    mk.op("tensor", lambda e, b=b, gi=gi: e.matmul(S[1][:, gi*128:(gi+1)*128], lhsT=wp[:, gi, :], rhs=mT[b][:, gi, :], start=True, stop=True), reads=[wp, mT[b]], writes=[S[1]])
                for gi in range(4):
                    mk.op("scalar", lambda e, gi=gi, tl=tl: e.activation(out=attnT[:, 4 + gi, tl*128:(tl+1)*128], in_=S[1][:, gi*128:(gi+1)*128], func=AF.Copy, scale=bs[:, gi:gi+1]), reads=[S[1], bs], writes=[attnT])
        for tl in range(16):
            t = chk * 16 + tl
            b = t % 2
            r0 = t * 128
            c0 = tl * 128
            mk.dma("sync", cx[b], lambda e, b=b, r0=r0: e.dma_start(out=xt[b][:], in_=x_src[r0:r0+128, :]), writes=[xt[b]])
            for nb in range(2):
                for fc in range(8):
                    mk.op("tensor", lambda e, nb=nb, fc=fc, c0=c0: e.matmul(O[nb][:], lhsT=attnT[:, fc, c0:c0+128], rhs=wo[:, fc, nb*512:(nb+1)*512], start=(fc == 0), stop=(fc == 7)), reads=[attnT, wo], writes=[O[nb]])
                mk.op("vector", lambda e, nb=nb, b=b: e.tensor_tensor(out=xt[b][:, nb*512:(nb+1)*512], in0=O[nb][:], in1=xt[b][:, nb*512:(nb+1)*512], op=ALU.add), reads=[O[nb], xt[b]], writes=[xt[b]])
            mk.dma("gpsimd", so[b], lambda e, b=b, r0=r0: e.dma_start(out=D["xbuf"][r0:r0+128, :], in_=xt[b][:]), reads=[xt[b]])
            mk.op("scalar", lambda e, b=b: e.activation(out=sq[:], in_=xt[b][:], func=AF.Square, accum_out=st[b][:, 0:1]), reads=[xt[b]], writes=[sq, st[b]])
            mk.op("scalar", lambda e, b=b: e.activation(out=st[b][:, 1:2], in_=st[b][:, 0:1], func=AF.Sqrt, scale=1.0/1024, bias=1e-6), reads=[st[b]], writes=[st[b]])
            mk.op("vector", lambda e, b=b: e.reciprocal(out=st[b][:, 2:3], in_=st[b][:, 1:2]), reads=[st[b]], writes=[st[b]])
            mk.op("vector", lambda e, b=b: e.scalar_tensor_tensor(out=h2f[b][:], in0=xt[b][:], scalar=st[b][:, 2:3], in1=gt[:], op0=ALU.mult, op1=ALU.mult), reads=[xt[b], st[b], gt], writes=[h2f[b]])
            mk.op("scalar", lambda e, b=b: e.copy(out=h2b[b][:], in_=h2f[b][:]), reads=[h2f[b]], writes=[h2b[b]])
            mk.dma("gpsimd", so[b], lambda e, b=b, r0=r0: e.dma_start(out=D["h2buf"][r0:r0+128, :], in_=h2b[b][:]), reads=[h2b[b]])
            for kc in range(8):
                dst = S[kc // 4]
                mk.op("tensor", lambda e, b=b, kc=kc, dst=dst: e.transpose(out=dst[:, (kc % 4)*128:(kc % 4 + 1)*128], in_=h2f[b][:, kc*128:(kc+1)*128], identity=idf[:]), reads=[h2f[b], idf], writes=[dst])
            for hf in range(2):
                mk.op("vector", lambda e, b=b, hf=hf: e.tensor_copy(out=h2T[b][:, hf*4:(hf+1)*4, :].rearrange("p a t -> p (a t)"), in_=S[hf][:]), reads=[S[hf]], writes=[h2T[b]])
            for kc in range(8):
                mk.op("tensor", lambda e, b=b, kc=kc: e.matmul(X[:, 0:16], lhsT=h2T[b][:, kc, :], rhs=wr[:, kc, :], start=(kc == 0), stop=(kc == 7)), reads=[h2T[b], wr], writes=[X])
            mk.op("scalar", lambda e, b=b: e.activation(out=ex[b][:], in_=X[:, 0:16], func=AF.Exp, accum_out=st[b][:, 3:4]), reads=[X], writes=[ex[b], st[b]])
            mk.op("vector", lambda e, b=b: e.reciprocal(out=st[b][:, 3:4], in_=st[b][:, 3:4]), reads=[st[b]], writes=[st[b]])
            mk.op("vector", lambda e, b=b, t=t: e.tensor_scalar(out=aff_sb[:, t, :], in0=ex[b][:], scalar1=st[b][:, 3:4], scalar2=None, op0=ALU.mult), reads=[ex[b], st[b]], writes=[aff_sb])
    mk.end_stage()


NIT = 26

def stage_C(mk, i, D, G):
    NE = 16
    mk.begin_stage()
    cc = mk.chan(); ccb = mk.chan(); csc = mk.chan()
    cgr = [mk.chan() for _ in range(2)]
    cxg = [mk.chan() for _ in range(2)]
    cwt = [mk.chan() for _ in range(2)]
    PS = G["PS"]; TP = G["TP"]
    Gp = PS[0:2]; U = PS[2:4]; Y = PS[4:7]
    idb = G["idb"]; cst = G["cst"]; iot = G["iot"]; aff_sb = G["aff_sb"]
    tri_incl, tri_excl, ones = cst[:, 0, :], cst[:, 1, :], cst[:, 2, :]
    A = aff_sb[:].rearrange("p j e -> p e j")
    cbuf = D["cbuf"]; h2buf = D["h2buf"]; xbuf = D["xbuf"]
    wg = D["moe_w_gate"]; wu = D["moe_w_up"]; wd = D["moe_w_down"]
    lo = mk.sb("lo", [128, NE], F32)
    thr = mk.sb("thr", [128, NE], F32)
    cnt = mk.sb("cnt", [128, NE], F32)
    ge = mk.sb("ge", [128, NE], F32)
    m3 = mk.sb("m3", [128, NE, 64], F32)
    mk.op("vector", lambda e: e.memset(lo[:], 0.0), writes=[lo])
    for it in range(NIT):
        step = 2.0 ** -(it + 1)
        mk.op("vector", lambda e, step=step: e.tensor_scalar(out=thr[:], in0=lo[:], scalar1=step, scalar2=None, op0=ALU.add), reads=[lo], writes=[thr])
        mk.op("vector", lambda e: e.tensor_tensor(out=m3[:], in0=A, in1=thr[:].unsqueeze(2).broadcast_to([128, NE, 64]), op=ALU.is_ge), reads=[aff_sb, thr], writes=[m3])
        mk.op("vector", lambda e: e.tensor_reduce(out=cnt[:], in_=m3[:], axis=AX.X, op=ALU.add), reads=[m3], writes=[cnt])
        mk.op("tensor", lambda e: e.matmul(Gp[0][:, 0:NE], lhsT=ones, rhs=cnt[:], start=True, stop=True), reads=[cst, cnt], writes=[Gp[0]])
        mk.op("vector", lambda e, step=step: e.tensor_scalar(out=ge[:], in0=Gp[0][:, 0:NE], scalar1=1024.0, scalar2=step, op0=ALU.is_ge, op1=ALU.mult), reads=[Gp[0]], writes=[ge])
        mk.op("vector", lambda e: e.tensor_tensor(out=lo[:], in0=lo[:], in1=ge[:], op=ALU.add), reads=[lo, ge], writes=[lo])
    crs = mk.sb("crs", [128, NE, 128], F32)
    cin = mk.sb("cin", [128, NE, 64], F32)
    tot = mk.sb("tot", [128, NE], F32)
    offx = mk.sb("offx", [128, NE], F32)
    totb = mk.sb("totb", [128, 128], F32)
    offr = mk.sb("offr", [128, NE, 128], F32)
    mk.op("vector", lambda e: e.tensor_tensor(out=m3[:], in0=A, in1=lo[:].unsqueeze(2).broadcast_to([128, NE, 64]), op=ALU.is_ge), reads=[aff_sb, lo], writes=[m3])
    mk.op("vector", lambda e: e.tensor_tensor(out=crs[:, :, 64:128], in0=A, in1=m3[:], op=ALU.mult), reads=[aff_sb, m3], writes=[crs])
    for p in range(NE):
        mk.op("vector", lambda e, p=p: e.tensor_tensor_scan(out=cin[:, p, :], data0=ones[:, 0:64], data1=m3[:, p, :], initial=0.0, op0=ALU.mult, op1=ALU.add), reads=[cst, m3], writes=[cin])
    mk.op("vector", lambda e: e.tensor_copy(out=tot[:], in_=cin[:, :, 63]), reads=[cin], writes=[tot])
    mk.op("tensor", lambda e: e.matmul(Gp[0][:, 0:NE], lhsT=tri_excl, rhs=tot[:], start=True, stop=True), reads=[cst, tot], writes=[Gp[0]])
    mk.op("vector", lambda e: e.tensor_copy(out=offx[:], in_=Gp[0][:, 0:NE]), reads=[Gp[0]], writes=[offx])
    mk.op("vector", lambda e: e.tensor_tensor(out=crs[:, :, 0:64], in0=cin[:], in1=offx[:].unsqueeze(2).broadcast_to([128, NE, 64]), op=ALU.add), reads=[cin, offx], writes=[crs])
    for p in range(NE):
        mk.op("vector", lambda e, p=p: e.tensor_copy(out=totb[:], in_=tot[:, p:p+1].broadcast_to([128, 128])), reads=[tot], writes=[totb])
        mk.op("tensor", lambda e, p=p: e.matmul(Gp[1][:, 0:128], lhsT=totb[:], rhs=tri_incl, start=True, stop=True), reads=[totb, cst], writes=[Gp[1]])
        mk.op("vector", lambda e, p=p: e.tensor_copy(out=offr[:, p, :], in_=Gp[1][:, 0:128]), reads=[Gp[1]], writes=[offr])
    Tcb = T("cbuf", cbuf)
    for q4 in range(4):
        mk.dma("sync", ccb, lambda e, q4=q4: e.dma_start(out=cbuf[q4*512:(q4+1)*512, :].rearrange("(p q) c -> q p c", q=128), in_=crs[:, q4*4:(q4+1)*4, :]), reads=[crs], writes=[Tcb])
    mk.seal(ccb, [Tcb])
    NC_ = NE * 8
    pc = mk.sb("pc", [128, NC_], F32)
    pcf = mk.sb("pcf", [128, NC_], F32)
    pci = mk.sb("pci", [128, NC_], I32)
    c2 = mk.sb("c2", [128, NC_], F32)
    gate = mk.sb("gate", [128, NC_], F32)
    tif = mk.sb("tif", [128, NC_], F32)
    tii = mk.sb("tii", [128, NC_], I32)
    junk = mk.sb("junk", [128, 128], F32)
    crow = [mk.sb(f"crow{k}", [128, 128], F32) for k in range(2)]
    for p in range(NE):
        for sb in range(8):
            col = p * 8 + sb
            mk.op("vector", lambda e, p=p, sb=sb, col=col: e.tensor_scalar(out=junk[:], in0=offr[:, p, :], scalar1=iot[:, sb:sb+1], scalar2=0.0, op0=ALU.is_le, op1=ALU.add, accum_out=pc[:, col:col+1]), reads=[offr, iot], writes=[junk, pc])
    mk.op("vector", lambda e: e.tensor_copy(out=pcf[:], in_=pc[:]), reads=[pc], writes=[pcf])
    for p in range(1, NE):
        mk.op("vector", lambda e, p=p: e.tensor_scalar(out=pcf[:, p*8:(p+1)*8], in0=pcf[:, p*8:(p+1)*8], scalar1=float(p * 128), scalar2=None, op0=ALU.add), reads=[pcf], writes=[pcf])
    mk.op("vector", lambda e: e.tensor_copy(out=pci[:], in_=pcf[:]), reads=[pcf], writes=[pci])
    for p in range(NE):
        for sb in range(8):
            col = p * 8 + sb
            cb = col % 2
            mk.dma("gpsimd", cgr[cb], lambda e, cb=cb, col=col: e.indirect_dma_start(out=crow[cb][:], out_offset=None, in_=cbuf[:, :], in_offset=bass.IndirectOffsetOnAxis(ap=pci[:, col:col+1], axis=0)), reads=[pci, Tcb], writes=[crow[cb]])
            mk.op("vector", lambda e, cb=cb, sb=sb, col=col: e.tensor_scalar(out=junk[:, 0:64], in0=crow[cb][:, 0:64], scalar1=iot[:, sb:sb+1], scalar2=0.0, op0=ALU.is_le, op1=ALU.add, accum_out=c2[:, col:col+1]), reads=[crow[cb], iot], writes=[junk, c2])
            mk.op("vector", lambda e, cb=cb, sb=sb, col=col: e.scalar_tensor_tensor(out=junk[:, 64:128], in0=crow[cb][:, 0:64], scalar=iot[:, 8+sb:9+sb], in1=crow[cb][:, 64:128], op0=ALU.is_equal, op1=ALU.mult, accum_out=gate[:, col:col+1]), reads=[crow[cb], iot], writes=[junk, gate])
    mk.op("vector", lambda e: e.scalar_tensor_tensor(out=tif[:], in0=c2[:], scalar=128.0, in1=pc[:], op0=ALU.mult, op1=ALU.add), reads=[pc, c2], writes=[tif])
    mk.op("vector", lambda e: e.tensor_copy(out=tii[:], in_=tif[:]), reads=[tif], writes=[tii])
    Th2 = T("h2buf", h2buf)
    Tx = T("xbuf", xbuf)
    xg = [mk.sb(f"xg{k}", [128, 1024], BF16) for k in range(2)]
    xsT2 = [mk.sb(f"xsT{k}", [128, 8, 1024], BF16) for k in range(2)]
    wgb = [mk.sb(f"wgb{k}", [128, 8, 256], BF16) for k in range(2)]
    wub = [mk.sb(f"wub{k}", [128, 8, 256], BF16) for k in range(2)]
    wdf = mk.sb("wdf", [128, 16, 1024], BF16)
    actF = mk.sb("actF", [128, 16, 1024], BF16)
    Tw = [T(f"wdf{k}", wdf.ap[:, 2*k:2*k+2, :]) for k in range(8)]
    Ta = [T(f"actF{k}", actF.ap[:, 2*k:2*k+2, :]) for k in range(8)]
    sg = [mk.sb(f"sg{k}", [128, 512], F32) for k in range(2)]
    yo = [mk.sb(f"yo{k}", [128, 1024], F32) for k in range(2)]
    def gather(p):
        xs = xsT2[p % 2]
        for sb in range(8):
            col = p * 8 + sb
            xb = col % 2
            mk.dma("gpsimd", cxg[xb], lambda e, xb=xb, col=col: e.indirect_dma_start(out=xg[xb][:], out_offset=None, in_=h2buf[:, :], in_offset=bass.IndirectOffsetOnAxis(ap=tii[:, col:col+1], axis=0)), reads=[tii, Th2], writes=[xg[xb]])
            for dc in range(8):
                mk.op("tensor", lambda e, xb=xb, dc=dc: e.transpose(out=TP[:, dc, :], in_=xg[xb][:, dc*128:(dc+1)*128], identity=idb[:]), reads=[xg[xb], idb], writes=[TP])
            mk.op("scalar", lambda e, sb=sb: e.copy(out=xs[:, :, sb*128:(sb+1)*128], in_=TP[:]), reads=[TP], writes=[xs])

    def gu(p):
        ew = i * 16 + p
        xs = xsT2[p % 2]
        for fg in range(8):
            wb = (p * 8 + fg) % 2
            f0 = fg * 256
            mk.dma("gpsimd", cwt[wb], lambda e, wb=wb, ew=ew, f0=f0: e.dma_start(out=wgb[wb][:], in_=wg[ew, :, f0:f0+256].rearrange("(dc p) f -> p dc f", p=128)), writes=[wgb[wb]])
            mk.dma("gpsimd", cwt[wb], lambda e, wb=wb, ew=ew, f0=f0: e.dma_start(out=wub[wb][:], in_=wu[ew, :, f0:f0+256].rearrange("(dc p) f -> p dc f", p=128)), writes=[wub[wb]])
            mk.dma("gpsimd", cwt[wb], lambda e, fg=fg, ew=ew, f0=f0: e.dma_start(out=wdf[:, 2*fg:2*fg+2, :], in_=wd[ew, f0:f0+256, :].rearrange("(fc p) d -> p fc d", p=128)), writes=[Tw[fg]])
            mk.seal(cwt[wb], [wgb[wb], wub[wb], Tw[fg]])
            k_ = 0
            for fc in range(2):
                for tb in range(2):
                    gb = Gp[k_ % 2]; ub = U[k_ % 2]; sgb = sg[k_ % 2]; k_ += 1
                    for dc in range(8):
                        mk.op("tensor", lambda e, gb=gb, wb=wb, dc=dc, fc=fc, tb=tb: e.matmul(gb[:], lhsT=wgb[wb][:, dc, fc*128:(fc+1)*128], rhs=xs[:, dc, tb*512:(tb+1)*512], start=(dc == 0), stop=(dc == 7)), reads=[wgb[wb], xs], writes=[gb])
                    for dc in range(8):
                        mk.op("tensor", lambda e, ub=ub, wb=wb, dc=dc, fc=fc, tb=tb: e.matmul(ub[:], lhsT=wub[wb][:, dc, fc*128:(fc+1)*128], rhs=xs[:, dc, tb*512:(tb+1)*512], start=(dc == 0), stop=(dc == 7)), reads=[wub[wb], xs], writes=[ub])
                    mk.op("scalar", lambda e, gb=gb, sgb=sgb: e.activation(out=sgb[:], in_=gb[:], func=AF.Silu), reads=[gb], writes=[sgb])
                    mk.op("vector", lambda e, ub=ub, sgb=sgb, fg=fg, fc=fc, tb=tb: e.tensor_tensor(out=actF[:, 2*fg+fc, tb*512:(tb+1)*512], in0=ub[:], in1=sgb[:], op=ALU.mult), reads=[ub, sgb], writes=[Ta[fg]])

    def down(p):
        yk = 0
        for tt in range(8):
            col = p * 8 + tt
            ob = yo[tt % 2]
            for nb in range(2):
                yb = Y[yk % 3]; yk += 1
                for fc in range(16):
                    mk.op("tensor", lambda e, yb=yb, fc=fc, tt=tt, nb=nb: e.matmul(yb[:], lhsT=actF[:, fc, tt*128:(tt+1)*128], rhs=wdf[:, fc, nb*512:(nb+1)*512], start=(fc == 0), stop=(fc == 15)), reads=[Ta[fc // 2], Tw[fc // 2]], writes=[yb])
                mk.op("vector", lambda e, yb=yb, ob=ob, nb=nb, col=col: e.tensor_scalar(out=ob[:, nb*512:(nb+1)*512], in0=yb[:], scalar1=gate[:, col:col+1], scalar2=None, op0=ALU.mult), reads=[yb, gate], writes=[ob])
            mk.dma("gpsimd", csc, lambda e, ob=ob, col=col: e.indirect_dma_start(out=xbuf[:, :], out_offset=bass.IndirectOffsetOnAxis(ap=tii[:, col:col+1], axis=0), in_=ob[:], in_offset=None, compute_op=ALU.add), reads=[ob, tii], writes=[Tx])

    gather(0)
    for p in range(NE):
        gu(p)
        if p + 1 < NE:
            gather(p + 1)
        down(p)
    mk.end_stage()


def stage_F(mk, D, G):
    mk.begin_stage()
    cc = mk.chan()
    ldx = [mk.chan() for _ in range(2)]
    sty = [mk.chan() for _ in range(2)]
    gt = mk.sb("gt", [128, 1024], F32)
    mk.dma("sync", cc, lambda e: e.dma_start(out=gt[:], in_=D["norm_final"][0:1, :].partition_broadcast(128)), writes=[gt])
    xt = [mk.sb(f"xt{k}", [128, 1024], F32) for k in range(2)]
    yt = [mk.sb(f"yt{k}", [128, 1024], F32) for k in range(2)]
    sq = mk.sb("sq", [128, 1024], F32)
    st = [mk.sb(f"st{k}", [128, 4], F32) for k in range(2)]
    final = []
    for t in range(64):
        b = t % 2
        r0 = t * 128
        mk.dma("sync", ldx[b], lambda e, b=b, r0=r0: e.dma_start(out=xt[b][:], in_=D["xbuf"][r0:r0+128, :]), writes=[xt[b]])
        mk.op("scalar", lambda e, b=b: e.activation(out=sq[:], in_=xt[b][:], func=AF.Square, accum_out=st[b][:, 0:1]), reads=[xt[b]], writes=[sq, st[b]])
        mk.op("scalar", lambda e, b=b: e.activation(out=st[b][:, 1:2], in_=st[b][:, 0:1], func=AF.Sqrt, scale=1.0/1024, bias=1e-6), reads=[st[b]], writes=[st[b]])
        mk.op("vector", lambda e, b=b: e.reciprocal(out=st[b][:, 2:3], in_=st[b][:, 1:2]), reads=[st[b]], writes=[st[b]])
        mk.op("vector", lambda e, b=b: e.scalar_tensor_tensor(out=yt[b][:], in0=xt[b][:], scalar=st[b][:, 2:3], in1=gt[:], op0=ALU.mult, op1=ALU.add if False else ALU.mult), reads=[xt[b], st[b], gt], writes=[yt[b]])
        final.append(mk.dma("gpsimd", sty[b], lambda e, b=b, r0=r0: e.dma_start(out=D["y"][r0:r0+128, :], in_=yt[b][:]), reads=[yt[b]]))
    for tok in final:
        mk.wait_tok("sync", tok)
    mk.end_stage()


def rope_cs(p, dim):
    inv = (10000.0 ** (-np.arange(0, dim, 2, dtype=np.float32) / dim)).astype(np.float32)
    ang = p.astype(np.float32)[:, None] * inv[None, :]
    return np.cos(ang).astype(np.float32), np.sin(ang).astype(np.float32)


def host_consts():
    pos = np.arange(SEQ)
    co, si = rope_cs(pos, 64)
    cs_even = np.ascontiguousarray(np.stack([co, si], 1))
    cr, sr = rope_cs(pos // 64, 64); cc, sc = rope_cs(pos % 64, 64)
    cs_odd = np.ascontiguousarray(np.stack([np.concatenate([cr, cc], 1), np.concatenate([sr, sc], 1)], 1))
    k = np.arange(128)[:, None]; q = np.arange(128)[None, :]
    masks = np.stack([(k >= q), (k <= q)], 1).astype(np.float32).astype(BF)
    Bm = np.zeros((128, 3, 4, 128), np.float32)
    Bh = np.zeros((16, 4, 128), np.float32)
    for gi, w in enumerate(POOL_WINDOWS):
        half = w // 2
        for var, t0 in enumerate((0, 1024, SEQ - 128)):
            for tl in range(128):
                Tt = t0 + tl
                lo = max(Tt - half, 0); hi = min(Tt + half, SEQ)
                cnt = hi - lo
                for s in range(lo, hi):
                    sl = s - t0
                    if 0 <= sl < 128:
                        Bm[sl, var, gi, tl] += 1.0 / cnt
                Bm[tl, var, gi, tl] -= 1.0
        for tl in range(128):
            for s in range(tl - half, tl + half):
                if s < 0:
                    Bh[s + 8, gi, tl] = 1.0 / w
                elif s >= 128:
                    Bh[8 + s - 128, gi, tl] = 1.0 / w
    cst = np.stack([(k <= q), (k < q), np.ones((128, 128)), np.eye(128)], 1).astype(np.float32)
    s = (np.arange(8)[None, :] * 128 + np.arange(128)[:, None]).astype(np.float32)
    iot = np.concatenate([s, s + 1], 1).astype(np.float32)
    return {"cs_even": cs_even, "cs_odd": cs_odd, "masks": masks, "Bm": Bm.astype(BF), "Bh": Bh.astype(BF),
            "cst": np.ascontiguousarray(cst), "iot": np.ascontiguousarray(iot)}


IN_SPECS = [
    ("x", [8192, 1024], F32), ("norm_mix", [4, 1024], F32), ("norm_ffn", [4, 1024], F32), ("norm_final", [1, 1024], F32),
    ("a_w_in", [2, 1024, 1280], F32), ("a_w_out", [2, 1024, 1024], F32), ("a_sink", [2, 8], F32),
    ("b_w_pool", [2, 4, 128, 128], F32), ("bsc", [2, 128, 4], F32), ("c_w_qkv", [2, 1024, 1536], F32),
    ("gain", [2, 1280], F32), ("c_w_out", [2, 1024, 1024], F32), ("moe_router", [4, 1024, 16], F32),
    ("moe_w_gate", [64, 1024, 2048], F32), ("moe_w_up", [64, 1024, 2048], F32), ("moe_w_down", [64, 2048, 1024], F32),
    ("cs_even", [8192, 2, 32], F32), ("cs_odd", [8192, 2, 64], F32), ("masks", [128, 2, 128], BF16),
    ("Bm", [128, 3, 4, 128], BF16), ("Bh", [16, 4, 128], BF16), ("cst", [128, 4, 128], F32), ("iot", [128, 16], F32),
]


def build_fused(depth=4):
    nc = bass.Bass("TRN2", target_bir_lowering=False)
    D = {}
    for name, shape, dt in IN_SPECS:
        D[name] = nc.dram_tensor(name, shape, dt, kind="ExternalInput").ap()
    D["y"] = nc.dram_tensor("y", [8192, 1024], F32, kind="ExternalOutput").ap()
    for name, shape, dt in [("xbuf", [8192, 1024], F32), ("h2buf", [8192, 1024], BF16), ("Ubuf", [8192, 512], BF16),
                            ("QT_o", [2, 64, 128, 512], BF16), ("QT_e", [2, 64, 64, 512], BF16),
                            ("KT_o", [2, 128, 8192], BF16), ("KT_e", [2, 64, 8192], BF16),
                            ("VA_o", [2, 64, 128, 128], BF16), ("VA_e", [2, 64, 128, 65], BF16),
                            ("cbuf", [17 * 128, 128], F32)]:
        D[name] = nc.dram_tensor(name, shape, dt).ap()
    with ExitStack() as es:
        mk = MK(nc, es)
        G = {}
        G["PS"] = [mk.ps(f"PS{k}", [128, 512], F32) for k in range(7)]
        G["TP"] = mk.ps("TP", [128, 8, 128], BF16)
        G["cst"] = mk.sb("cstg", [128, 4, 128], F32, glob=True)
        G["iot"] = mk.sb("iotg", [128, 16], F32, glob=True)
        G["idb"] = mk.sb("idb", [128, 128], BF16, glob=True)
        G["aff_sb"] = mk.sb("aff_sb", [128, 64, 16], F32, glob=True)
        G["onesb"] = mk.sb("onesb", [128, 128], BF16, glob=True)
        G["idf"] = T("idf", G["cst"].ap[:, 3, :])
        mk.begin_stage()
        c0 = mk.chan()
        mk.dma("sync", c0, lambda e: e.dma_start(out=G["cst"][:], in_=D["cst"][:]), writes=[G["cst"]])
        mk.dma("sync", c0, lambda e: e.dma_start(out=G["iot"][:], in_=D["iot"][:]), writes=[G["iot"]])
        mk.seal(c0, [G["cst"], G["iot"]])
        mk.op("vector", lambda e: e.tensor_copy(out=G["idb"][:], in_=G["cst"][:, 3, :]), reads=[G["cst"]], writes=[G["idb"]])
        mk.op("vector", lambda e: e.tensor_copy(out=G["onesb"][:], in_=G["cst"][:, 2, :]), reads=[G["cst"]], writes=[G["onesb"]])
        mk.end_stage()
        for i in range(depth):
            stage_A(mk, i, D, G)
            stage_B(mk, i, D, G)
            stage_C(mk, i, D, G)
        stage_F(mk, D, G)
    return nc


def kernel(x, norm_mix, norm_ffn, norm_final, a_w_in, a_w_out, a_sink, b_w_pool, b_scale,
           c_w_qkv, c_q_norm, c_k_norm, c_w_out, moe_router, moe_w_gate, moe_w_up, moe_w_down):
    f = lambda a: np.ascontiguousarray(np.asarray(a, dtype=np.float32))
    hc = host_consts()
    c_q_norm = f(c_q_norm); c_k_norm = f(c_k_norm); b_scale = f(b_scale)
    shared = {
        "norm_mix": f(norm_mix), "norm_ffn": f(norm_ffn), "norm_final": f(norm_final).reshape(1, 1024),
        "a_w_in": f(a_w_in), "a_w_out": f(a_w_out), "a_sink": f(a_sink), "b_w_pool": f(b_w_pool),
        "bsc": np.ascontiguousarray(b_scale.reshape(2, 4, 128).transpose(0, 2, 1)),
        "c_w_qkv": f(c_w_qkv),
        "gain": np.ascontiguousarray(np.stack([np.concatenate([np.tile(c_q_norm[j], 8), np.tile(c_k_norm[j], 2)]) for j in range(2)], 0)),
        "c_w_out": f(c_w_out), "moe_router": f(moe_router),
        "moe_w_gate": f(moe_w_gate).reshape(64, 1024, 2048), "moe_w_up": f(moe_w_up).reshape(64, 1024, 2048),
        "moe_w_down": f(moe_w_down).reshape(64, 2048, 1024),
    }
    shared.update(hc)
    x = f(x)
    nc = build_fused()
    in_maps = []
    for b in range(2):
        d = dict(shared)
        d["x"] = np.ascontiguousarray(x[b])
        in_maps.append(d)
    res = run_bass_kernel_spmd(nc, in_maps, core_ids=[0, 1])
    return np.stack([res.results[b]["y"] for b in range(2)], 0)
```

```python
import numpy as np
import ml_dtypes
import concourse.bass as bass
import concourse.mybir as mybir
from concourse.bass_utils import run_bass_kernel_spmd
from contextlib import ExitStack

F32 = mybir.dt.float32
BF16 = mybir.dt.bfloat16
I32 = mybir.dt.int32
AF = mybir.ActivationFunctionType
ALU = mybir.AluOpType
AX = mybir.AxisListType
BF = ml_dtypes.bfloat16
SEQ = 8192
POOL_WINDOWS = (2, 4, 8, 16)


class T:
    def __init__(self, name, ap):
        self.name = name
        self.ap = ap
        self.w = None
        self.r = []
    def __getitem__(self, k):
        return self.ap[k]


class Chan:
    def __init__(self, name, sem, step):
        self.name, self.sem, self.step, self.val = name, sem, step, 0


class MK:
    ENGS = ["tensor", "vector", "scalar", "gpsimd", "sync"]

    def __init__(self, nc, es):
        self.nc = nc
        self.es = es
        self.ses = None
        self.q = {e: [] for e in self.ENGS}
        self.prog = {}
        for e in self.ENGS:
            s = es.enter_context(nc.semaphore("p_" + e))
            self.prog[e] = Chan(e, s, 1)
        self.waited = {e: {} for e in self.ENGS}
        self.pool = []
        self.pool_i = 0
        self.uid = 0

    def begin_stage(self):
        self.ses = ExitStack()
        self.pool_i = 0
        chans = list(self.prog.values()) + self.pool
        for eng in self.ENGS:
            for ch in chans:
                if ch.val > 0 and self.waited[eng].get(ch.name, -1) < ch.val:
                    self.waited[eng][ch.name] = ch.val
                    self.q[eng].append(lambda e, s=ch.sem, v=ch.val: e.wait_ge(s, v))

    def end_stage(self):
        self.flush()
        self.ses.close()
        self.ses = None

    def flush(self):
        nc = self.nc
        q = self.q
        with nc.Block() as block:
            @block.tensor
            def _(e):
                for f in q["tensor"]: f(e)
            @block.vector
            def _(e):
                for f in q["vector"]: f(e)
            @block.scalar
            def _(e):
                for f in q["scalar"]: f(e)
            @block.gpsimd
            def _(e):
                for f in q["gpsimd"]: f(e)
            @block.sync
            def _(e):
                for f in q["sync"]: f(e)
        self.q = {e: [] for e in self.ENGS}

    def chan(self, name=None):
        if self.pool_i < len(self.pool):
            ch = self.pool[self.pool_i]
        else:
            s = self.es.enter_context(self.nc.semaphore("c_%d" % len(self.pool)))
            ch = Chan("c_%d" % len(self.pool), s, 16)
            self.pool.append(ch)
        self.pool_i += 1
        return ch

    def sb(self, name, shape, dt, glob=False):
        self.uid += 1
        st = self.es if (glob or self.ses is None) else self.ses
        t = st.enter_context(self.nc.sbuf_tensor("%s_%d" % (name, self.uid), list(shape), dt))
        return T(name, t)

    def ps(self, name, shape, dt=F32):
        t = self.es.enter_context(self.nc.psum_tensor(name, list(shape), dt))
        return T(name, t)

    def _deps(self, eng, reads, writes):
        deps = {}
        def add(tok):
            if tok is None:
                return
            ch, v = tok
            if ch.name == eng and eng == "tensor":
                return
            if deps.get(ch.name, (None, -1))[1] < v:
                deps[ch.name] = (ch, v)
        for t in reads:
            add(t.w)
        for t in writes:
            add(t.w)
            for r in t.r:
                add(r)
        out = []
        for name, (ch, v) in deps.items():
            if self.waited[eng].get(name, -1) >= v:
                continue
            self.waited[eng][name] = v
            out.append((ch.sem, v))
        return out

    def _record(self, tok, reads, writes):
        for t in reads:
            t.r.append(tok)
            if len(t.r) > 64:
                best = {}
                for (c, v) in t.r:
                    if best.get(c.name, (None, -1))[1] < v:
                        best[c.name] = (c, v)
                t.r = list(best.values())
        for t in writes:
            t.w = tok
            t.r = []

    def op(self, eng, fn, reads=(), writes=()):
        waits = self._deps(eng, reads, writes)
        ch = self.prog[eng]
        ch.val += 1
        tok = (ch, ch.val)
        sem = ch.sem
        def run(e, waits=waits, fn=fn, sem=sem):
            for (s, v) in waits:
                e.wait_ge(s, v)
            fn(e).then_inc(sem, 1)
        self.q[eng].append(run)
        self._record(tok, reads, writes)
        return tok

    def dma(self, eng, ch, fn, reads=(), writes=()):
        waits = self._deps(eng, reads, writes)
        ch.val += 16
        tok = (ch, ch.val)
        sem = ch.sem
        def run(e, waits=waits, fn=fn, sem=sem):
            for (s, v) in waits:
                e.wait_ge(s, v)
            fn(e).then_inc(sem, 16)
        self.q[eng].append(run)
        self._record(tok, reads, writes)
        return tok

    def seal(self, ch, tiles):
        for t in tiles:
            t.w = (ch, ch.val)

    def wait_tok(self, eng, tok):
        ch, v = tok
        if self.waited[eng].get(ch.name, -1) >= v:
            return
        self.waited[eng][ch.name] = v
        self.q[eng].append(lambda e, s=ch.sem, v=v: e.wait_ge(s, v))


def stage_A(mk, i, D, G):
    even = (i % 2 == 0)
    j = i // 2
    N = 1280 if even else 1536
    NT = 64
    x_src = D["x"] if i == 0 else D["xbuf"]
    w = D["a_w_in"][j] if even else D["c_w_qkv"][j]
    cs = D["cs_even"] if even else D["cs_odd"]
    HD = 64 if even else 128
    QT = D["QT_e"] if even else D["QT_o"]
    KT = D["KT_e"] if even else D["KT_o"]
    VA = D["VA_e"] if even else D["VA_o"]
    mk.begin_stage()
    cw = mk.chan(); cc = mk.chan()
    ldx = [mk.chan() for _ in range(2)]
    ldc = [mk.chan() for _ in range(2)]
    sto = [mk.chan() for _ in range(2)]
    idb = G["idb"]
    TP = G["TP"]; pp = G["PS"][0:3]
    wbf = mk.sb("wbf", [128, 8, N], BF16)
    gt = mk.sb("gt", [128, 1024], F32)
    for kc in range(8):
        mk.dma("gpsimd", cw, lambda e, kc=kc: e.dma_start(out=wbf[:, kc, :], in_=w[kc*128:(kc+1)*128, :]), writes=[wbf])
    mk.dma("sync", cc, lambda e: e.dma_start(out=gt[:], in_=D["norm_mix"][i:i+1, :].partition_broadcast(128)), writes=[gt])
    if not even:
        gn = mk.sb("gn", [128, 1280], F32)
        mk.dma("sync", cc, lambda e: e.dma_start(out=gn[:], in_=D["gain"][j:j+1, :].partition_broadcast(128)), writes=[gn])
    mk.seal(cc, [gt] + ([] if even else [gn]))
    xt = [mk.sb(f"xt{k}", [128, 1024], F32) for k in range(2)]
    sq = mk.sb("sq", [128, 1024], F32)
    sqa = mk.sb("sqa", [128, 1024], F32)
    st = [mk.sb(f"st{k}", [128, 4], F32) for k in range(2)]
    hb = [mk.sb(f"hb{k}", [128, 1024], BF16) for k in range(2)]
    hT = [mk.sb(f"hT{k}", [128, 8, 128], BF16) for k in range(2)]
    pf = [mk.sb(f"pf{k}", [128, N], F32) for k in range(2)]
    ot = [mk.sb(f"ot{k}", [128, N], BF16) for k in range(2)]
    cst = [mk.sb(f"cst{k}", [128, 2, 32 if even else 64], F32) for k in range(2)]
    tmp = [mk.sb(f"tmp{k}", [128, 4, 640], F32) for k in range(2)]
    tq = [mk.sb(f"tq{k}", [128, 10, 128], BF16) for k in range(2)]
    VW = HD + 1 if even else HD
    vaug = [mk.sb(f"vaug{k}", [128, 2, VW], BF16) for k in range(2)]
    for k in range(2):
        mk.op("vector", lambda e, k=k: e.memset(vaug[k][:], 1.0), writes=[vaug[k]])
    if not even:
        ms = [mk.sb(f"ms{k}", [128, 16], F32) for k in range(2)]
    def front(t):
        b = t % 2
        r0 = t * 128
        mk.dma("sync", ldx[b], lambda e, b=b, r0=r0: e.dma_start(out=xt[b][:], in_=x_src[r0:r0+128, :]), writes=[xt[b]])
        mk.dma("sync", ldc[b], lambda e, b=b, r0=r0: e.dma_start(out=cst[b][:], in_=cs[r0:r0+128]), writes=[cst[b]])
        mk.op("scalar", lambda e, b=b: e.activation(out=sqa[:], in_=xt[b][:], func=AF.Square, accum_out=st[b][:, 0:1]), reads=[xt[b]], writes=[sqa, st[b]])
        mk.op("scalar", lambda e, b=b: e.activation(out=st[b][:, 1:2], in_=st[b][:, 0:1], func=AF.Sqrt, scale=1.0/1024, bias=1e-6), reads=[st[b]], writes=[st[b]])
        mk.op("vector", lambda e, b=b: e.reciprocal(out=st[b][:, 2:3], in_=st[b][:, 1:2]), reads=[st[b]], writes=[st[b]])
        mk.op("vector", lambda e, b=b: e.scalar_tensor_tensor(out=hb[b][:], in0=xt[b][:], scalar=st[b][:, 2:3], in1=gt[:], op0=ALU.mult, op1=ALU.mult), reads=[xt[b], st[b], gt], writes=[hb[b]])
        for kc in range(8):
            mk.op("tensor", lambda e, b=b, kc=kc: e.transpose(out=TP[:, kc, :], in_=hb[b][:, kc*128:(kc+1)*128], identity=idb[:]), reads=[hb[b], idb], writes=[TP])
        mk.op("scalar", lambda e, b=b: e.copy(out=hT[b][:], in_=TP[:]), reads=[TP], writes=[hT[b]])
        nblocks = [(n0, min(512, N - n0)) for n0 in range(0, N, 512)]
        for bi, (n0, nw) in enumerate(nblocks):
            for kc in range(8):
                mk.op("tensor", lambda e, b=b, kc=kc, bi=bi, n0=n0, nw=nw: e.matmul(pp[bi][:, 0:nw], lhsT=hT[b][:, kc, :], rhs=wbf[:, kc, n0:n0+nw], start=(kc == 0), stop=(kc == 7)), reads=[hT[b], wbf], writes=[pp[bi]])
            mk.op("scalar", lambda e, b=b, bi=bi, n0=n0, nw=nw: e.copy(out=pf[b][:, n0:n0+nw], in_=pp[bi][:, 0:nw]), reads=[pp[bi]], writes=[pf[b]])

    def back(t):
        b = t % 2
        r0 = t * 128
        NH = 10
        if even:
            v4 = pf[b][:, 0:640].rearrange("p (h two j) -> p h two j", two=2, j=32)
            o4 = ot[b][:, 0:640].rearrange("p (h two j) -> p h two j", two=2, j=32)
            x1, x2 = v4[:, :, 0, :], v4[:, :, 1, :]
            o1, o2 = o4[:, :, 0, :], o4[:, :, 1, :]
            cosb = cst[b][:, 0, :].unsqueeze(1).broadcast_to([128, NH, 32])
            sinb = cst[b][:, 1, :].unsqueeze(1).broadcast_to([128, NH, 32])
            tm = [tmp[b][:, k, 0:NH*32].rearrange("p (h j) -> p h j", j=32) for k in range(4)]
        else:
            v3 = pf[b][:, 0:1280].rearrange("p (h d) -> p h d", d=128)
            mk.op("vector", lambda e, b=b: e.tensor_tensor(out=sq[:, 0:1024], in0=pf[b][:, 0:1024], in1=pf[b][:, 0:1024], op=ALU.mult), reads=[pf[b]], writes=[sq])
            mk.op("vector", lambda e, b=b: e.tensor_reduce(out=ms[b][:, 0:8], in_=sq[:, 0:1024].rearrange("p (h d) -> p h d", d=128), axis=AX.X, op=ALU.add), reads=[sq], writes=[ms[b]])
            mk.op("vector", lambda e, b=b: e.tensor_tensor(out=sq[:, 0:256], in0=pf[b][:, 1024:1280], in1=pf[b][:, 1024:1280], op=ALU.mult), reads=[pf[b]], writes=[sq])
            mk.op("vector", lambda e, b=b: e.tensor_reduce(out=ms[b][:, 8:10], in_=sq[:, 0:256].rearrange("p (h d) -> p h d", d=128), axis=AX.X, op=ALU.add), reads=[sq], writes=[ms[b]])
            mk.op("scalar", lambda e, b=b: e.activation(out=ms[b][:, 0:10], in_=ms[b][:, 0:10], func=AF.Sqrt, scale=1.0/128, bias=1e-6), reads=[ms[b]], writes=[ms[b]])
            mk.op("vector", lambda e, b=b: e.reciprocal(out=ms[b][:, 0:10], in_=ms[b][:, 0:10]), reads=[ms[b]], writes=[ms[b]])
            mk.op("vector", lambda e, b=b, v3=v3: e.tensor_tensor(out=v3, in0=v3, in1=ms[b][:, 0:10].unsqueeze(2).broadcast_to([128, 10, 128]), op=ALU.mult), reads=[pf[b], ms[b]], writes=[pf[b]])
            mk.op("vector", lambda e, b=b: e.tensor_tensor(out=pf[b][:, 0:1280], in0=pf[b][:, 0:1280], in1=gn[:], op=ALU.mult), reads=[pf[b], gn], writes=[pf[b]])
            v5 = pf[b][:, 0:1280].rearrange("p (h a two j) -> p h a two j", a=2, two=2, j=32)
            o5 = ot[b][:, 0:1280].rearrange("p (h a two j) -> p h a two j", a=2, two=2, j=32)
            x1, x2 = v5[:, :, :, 0, :], v5[:, :, :, 1, :]
            o1, o2 = o5[:, :, :, 0, :], o5[:, :, :, 1, :]
            cosb = cst[b][:, 0, :].rearrange("p (a j) -> p a j", j=32).unsqueeze(1).broadcast_to([128, NH, 2, 32])
            sinb = cst[b][:, 1, :].rearrange("p (a j) -> p a j", j=32).unsqueeze(1).broadcast_to([128, NH, 2, 32])
            tm = [tmp[b][:, k, :].rearrange("p (h a j) -> p h a j", a=2, j=32) for k in range(4)]
        R = [pf[b], cst[b]]
        mk.op("vector", lambda e, x1=x1, cosb=cosb, tm=tm: e.tensor_tensor(out=tm[0], in0=x1, in1=cosb, op=ALU.mult), reads=R, writes=[tmp[b]])
        mk.op("gpsimd", lambda e, x2=x2, sinb=sinb, tm=tm: e.tensor_tensor(out=tm[1], in0=x2, in1=sinb, op=ALU.mult), reads=R, writes=[tmp[b]])
        mk.op("vector", lambda e, x1=x1, sinb=sinb, tm=tm: e.tensor_tensor(out=tm[2], in0=x1, in1=sinb, op=ALU.mult), reads=R, writes=[tmp[b]])
        mk.op("gpsimd", lambda e, x2=x2, cosb=cosb, tm=tm: e.tensor_tensor(out=tm[3], in0=x2, in1=cosb, op=ALU.mult), reads=R, writes=[tmp[b]])
        mk.op("vector", lambda e, o1=o1, tm=tm: e.tensor_tensor(out=o1, in0=tm[0], in1=tm[1], op=ALU.subtract), reads=[tmp[b]], writes=[ot[b]])
        mk.op("vector", lambda e, o2=o2, tm=tm: e.tensor_tensor(out=o2, in0=tm[2], in1=tm[3], op=ALU.add), reads=[tmp[b]], writes=[ot[b]])
        if even:
            mk.op("scalar", lambda e, b=b: e.copy(out=ot[b][:, 768:1280], in_=pf[b][:, 768:1280]), reads=[pf[b]], writes=[ot[b]])
            mk.op("scalar", lambda e, b=b: e.copy(out=vaug[b][:, :, 0:64], in_=pf[b][:, 640:768].rearrange("p (g d) -> p g d", d=64)), reads=[pf[b]], writes=[vaug[b]])
            for c in range(5):
                mk.op("tensor", lambda e, b=b, c=c: e.transpose(out=TP[:, c, :], in_=ot[b][:, c*128:(c+1)*128], identity=idb[:]), reads=[ot[b], idb], writes=[TP])
            mk.op("scalar", lambda e, b=b: e.copy(out=tq[b][:, 0:5, :], in_=TP[:, 0:5, :]), reads=[TP], writes=[tq[b]])
            for pr in range(2):
                for g in range(2):
                    dst = QT[g, t].rearrange("d (cc two qq) -> d cc two qq", two=2, qq=128)[:, :, pr, :]
                    mk.dma("sync", sto[b], lambda e, b=b, pr=pr, g=g, dst=dst: e.dma_start(out=dst, in_=tq[b][pr*64:(pr+1)*64, g*2:(g+1)*2, :]), reads=[tq[b]])
            for g in range(2):
                mk.dma("gpsimd", sto[b], lambda e, b=b, g=g, r0=r0: e.dma_start(out=KT[g, :, r0:r0+128], in_=tq[b][g*64:(g+1)*64, 4, :]), reads=[tq[b]])
            mk.dma("gpsimd", sto[b], lambda e, b=b, r0=r0: e.dma_start(out=D["Ubuf"][r0:r0+128, :], in_=ot[b][:, 768:1280]), reads=[ot[b]])
        else:
            mk.op("scalar", lambda e, b=b: e.copy(out=vaug[b][:, :, 0:128], in_=pf[b][:, 1280:1536].rearrange("p (g d) -> p g d", d=128)), reads=[pf[b]], writes=[vaug[b]])
            for h in range(8):
                mk.op("tensor", lambda e, b=b, h=h: e.transpose(out=TP[:, h, :], in_=ot[b][:, h*128:(h+1)*128], identity=idb[:]), reads=[ot[b], idb], writes=[TP])
            mk.op("scalar", lambda e, b=b: e.copy(out=tq[b][:, 0:8, :], in_=TP[:]), reads=[TP], writes=[tq[b]])
            for g in range(2):
                mk.op("tensor", lambda e, b=b, g=g: e.transpose(out=TP[:, g, :], in_=ot[b][:, 1024+g*128:1024+(g+1)*128], identity=idb[:]), reads=[ot[b], idb], writes=[TP])
            mk.op("scalar", lambda e, b=b: e.copy(out=tq[b][:, 8:10, :], in_=TP[:, 0:2, :]), reads=[TP], writes=[tq[b]])
            for g in range(2):
                mk.dma("sync", sto[b], lambda e, b=b, g=g, t=t: e.dma_start(out=QT[g, t].rearrange("d (hh qq) -> d hh qq", qq=128), in_=tq[b][:, g*4:(g+1)*4, :]), reads=[tq[b]])
                mk.dma("gpsimd", sto[b], lambda e, b=b, g=g, r0=r0: e.dma_start(out=KT[g, :, r0:r0+128], in_=tq[b][:, 8+g, :]), reads=[tq[b]])
        mk.dma("gpsimd", sto[b], lambda e, b=b, t=t: e.dma_start(out=VA[:, t].rearrange("g kp c -> kp g c"), in_=vaug[b][:]), reads=[vaug[b]])

    front(0)
    for t in range(NT):
        if t + 1 < NT:
            front(t + 1)
        back(t)
    mk.end_stage()


def stage_B(mk, i, D, G):
    even = (i % 2 == 0)
    j = i // 2
    HD = 64 if even else 128
    NKT = 64
    x_src = D["x"] if i == 0 else D["xbuf"]
    QT = D["QT_e"] if even else D["QT_o"]
    KT = D["KT_e"] if even else D["KT_o"]
    VA = D["VA_e"] if even else D["VA_o"]
    w_out = D["a_w_out"][j] if even else D["c_w_out"][j]
    scale = HD ** -0.5
    mk.begin_stage()
    cc = mk.chan(); cw = mk.chan()
    ck = [mk.chan() for _ in range(2)]
    cq = [mk.chan() for _ in range(2)]
    cx = [mk.chan() for _ in range(2)]
    cu = [mk.chan() for _ in range(2)]
    so = [mk.chan() for _ in range(2)]
    idb = G["idb"]; idf = G["idf"]; aff_sb = G["aff_sb"]
    PS = G["PS"]; TP = G["TP"]
    S = PS[0:2]; O = PS[2:6]; X = PS[6]
    wo = mk.sb("wo", [128, 8, 1024], BF16)
    for kc in range(8):
        mk.dma("gpsimd", cw, lambda e, kc=kc: e.dma_start(out=wo[:, kc, :], in_=w_out[kc*128:(kc+1)*128, :]), writes=[wo])
    gt = mk.sb("gt", [128, 1024], F32)
    mk.dma("sync", cc, lambda e: e.dma_start(out=gt[:], in_=D["norm_ffn"][i:i+1, :].partition_broadcast(128)), writes=[gt])
    wr = mk.sb("wr", [128, 8, 16], F32)
    mk.dma("sync", cc, lambda e: e.dma_start(out=wr[:], in_=D["moe_router"][i].rearrange("(kc p) e -> p kc e", p=128)), writes=[wr])
    consts = [gt, wr]
    if even:
        msk = mk.sb("msk", [128, 2, 128], BF16)
        mk.dma("sync", cc, lambda e: e.dma_start(out=msk[:], in_=D["masks"][:]), writes=[msk])
        snk = mk.sb("snk", [128, 8], F32)
        mk.dma("sync", cc, lambda e: e.dma_start(out=snk[:], in_=D["a_sink"][j:j+1, :].partition_broadcast(128)), writes=[snk])
        bm = mk.sb("bm", [128, 3, 4, 128], BF16)
        bh = mk.sb("bh", [16, 4, 128], BF16)
        mk.dma("sync", cc, lambda e: e.dma_start(out=bm[:], in_=D["Bm"][:]), writes=[bm])
        mk.dma("sync", cc, lambda e: e.dma_start(out=bh[:], in_=D["Bh"][:]), writes=[bh])
        bs = mk.sb("bs", [128, 4], F32)
        mk.dma("sync", cc, lambda e: e.dma_start(out=bs[:], in_=D["bsc"][j]), writes=[bs])
        consts += [msk, snk, bm, bh, bs]
    mk.seal(cc, consts)
    if even:
        mk.op("scalar", lambda e: e.activation(out=snk[:], in_=snk[:], func=AF.Exp), reads=[snk], writes=[snk])
        wp = mk.sb("wp", [128, 4, 128], BF16)
        for gi in range(4):
            mk.dma("gpsimd", cw, lambda e, gi=gi: e.dma_start(out=wp[:, gi, :], in_=D["b_w_pool"][j, gi]), writes=[wp])
    attnT = mk.sb("attnT", [128, 8, 2048], BF16)
    ksb = [mk.sb(f"ksb{k}", [128, 8192], BF16) for k in range(2)]
    VW = HD + 1 if even else HD
    vsb = [mk.sb(f"vsb{k}", [128, NKT, VW], BF16) for k in range(2)]
    qsb = [mk.sb(f"qsb{k}", [128, 512], BF16) for k in range(2)]
    if even:
        for k in range(2):
            mk.op("gpsimd", lambda e, k=k: e.memset(ksb[k][:], 0.0), writes=[ksb[k]])
            mk.op("vector", lambda e, k=k: e.memset(qsb[k][:], 0.0), writes=[qsb[k]])
    pT = [mk.sb(f"pT{k}", [128, 512], BF16) for k in range(4)]
    asb = [mk.sb(f"asb{k}", [128, 512], BF16) for k in range(2)]
    rc = [mk.sb(f"rc{k}", [128, 4], F32) for k in range(2)]
    if not even:
        accA = mk.sb("accA", [128, 512], F32)
        accB = mk.sb("accB", [128, 512], F32)
        rinv = mk.sb("rinv", [128, 512], F32)
    for g in range(2):
        mk.dma("sync", ck[g], lambda e, g=g: e.dma_start(out=ksb[g][0:HD, :], in_=KT[g]), writes=[ksb[g]])
        for hf in range(4):
            mk.dma("sync", ck[g], lambda e, g=g, hf=hf: e.dma_start(out=vsb[g][:, hf*16:(hf+1)*16, :], in_=VA[g, hf*16:(hf+1)*16].rearrange("kt kp c -> kp kt c")), writes=[vsb[g]])
        mk.seal(ck[g], [ksb[g], vsb[g]])
    xt = [mk.sb(f"xt{k}", [128, 1024], F32) for k in range(2)]
    sq = mk.sb("sq", [128, 1024], F32)
    st = [mk.sb(f"st{k}", [128, 4], F32) for k in range(2)]
    h2f = [mk.sb(f"h2f{k}", [128, 1024], F32) for k in range(2)]
    h2b = [mk.sb(f"h2b{k}", [128, 1024], BF16) for k in range(2)]
    h2T = [mk.sb(f"h2T{k}", [128, 8, 128], F32) for k in range(2)]
    ex = [mk.sb(f"ex{k}", [128, 16], F32) for k in range(2)]
    if even:
        um = [mk.sb(f"um{k}", [128, 512], BF16) for k in range(2)]
        uh = [mk.sb(f"uh{k}", [16, 512], BF16) for k in range(2)]
        mT = [mk.sb(f"mT{k}", [128, 4, 128], BF16) for k in range(2)]
    qi = 0
    for chk in range(4):
        for g in range(2):
            for ql in range(16):
                qt = chk * 16 + ql
                qb = qi % 2; qi += 1
                mk.dma("sync", cq[qb], lambda e, qb=qb, g=g, qt=qt: e.dma_start(out=qsb[qb][0:HD, :], in_=QT[g, qt]), writes=[qsb[qb]])
                if not even:
                    nk = 64
                    LA = 2
                    OT = O[0]; RS = O[1]
                    S4 = [PS[0], PS[1], PS[4], PS[5]]
                    for ii in range(nk + LA):
                        if ii < nk:
                            kt = ii
                            sb_ = S4[ii % 4]; pb = pT[ii % 4]
                            mk.op("tensor", lambda e, sb_=sb_, g=g, kt=kt, qb=qb: e.matmul(sb_[:], lhsT=ksb[g][:, kt*128:(kt+1)*128], rhs=qsb[qb][:], start=True, stop=True), reads=[ksb[g], qsb[qb]], writes=[sb_])
                            mk.op("scalar", lambda e, sb_=sb_, pb=pb: e.activation(out=pb[:], in_=sb_[:], func=AF.Exp, scale=scale), reads=[sb_], writes=[pb])
                            if kt % 3 != 2:
                                if kt == 0:
                                    mk.op("vector", lambda e, pb=pb: e.tensor_copy(out=accA[:], in_=pb[:]), reads=[pb], writes=[accA])
                                else:
                                    mk.op("vector", lambda e, pb=pb: e.tensor_tensor(out=accA[:], in0=accA[:], in1=pb[:], op=ALU.add), reads=[pb, accA], writes=[accA])
                        if ii >= LA:
                            i2 = ii - LA
                            pb = pT[i2 % 4]
                            mk.op("tensor", lambda e, pb=pb, g=g, i2=i2, nk=nk: e.matmul(OT[:], lhsT=vsb[g][:, i2, :], rhs=pb[:], start=(i2 == 0), stop=(i2 == nk - 1)), reads=[pb, vsb[g]], writes=[OT])
                            if i2 % 3 == 2:
                                mk.op("tensor", lambda e, pb=pb, i2=i2: e.matmul(RS[:], lhsT=G["onesb"][:], rhs=pb[:], start=(i2 == 2), stop=False), reads=[pb, G["onesb"]], writes=[RS])
                    mk.op("tensor", lambda e: e.matmul(RS[:], lhsT=G["cst"][:, 2, :], rhs=accA[:], start=False, stop=True), reads=[G["cst"], accA], writes=[RS])
                    mk.op("vector", lambda e: e.reciprocal(out=rinv[:], in_=RS[:]), reads=[RS], writes=[rinv])
                    mk.op("vector", lambda e, g=g, ql=ql: e.tensor_tensor(out=attnT[:, g*4:(g+1)*4, ql*128:(ql+1)*128], in0=OT[:].rearrange("p (h q) -> p h q", q=128), in1=rinv[:].rearrange("p (h q) -> p h q", q=128), op=ALU.mult), reads=[OT, rinv], writes=[attnT])
                    continue
                if even:
                    kts = [(kt, kt - qt) for kt in (qt - 1, qt, qt + 1) if 0 <= kt < 64]
                else:
                    kts = [(kt, 0) for kt in range(64)]
                nk = len(kts)
                for ii in range(nk + 1):
                    if ii < nk:
                        kt, rel = kts[ii]
                        sb_ = S[ii % 2]; pb = pT[ii % 3]
                        mk.op("tensor", lambda e, sb_=sb_, g=g, kt=kt, qb=qb: e.matmul(sb_[:], lhsT=ksb[g][:, kt*128:(kt+1)*128], rhs=qsb[qb][:], start=True, stop=True), reads=[ksb[g], qsb[qb]], writes=[sb_])
                        mk.op("scalar", lambda e, sb_=sb_, pb=pb: e.activation(out=pb[:], in_=sb_[:], func=AF.Exp, scale=scale), reads=[sb_], writes=[pb])
                        if even and rel != 0:
                            mi = 0 if rel < 0 else 1
                            mk.op("vector", lambda e, pb=pb, mi=mi: e.tensor_tensor(out=pb[:].rearrange("p (h q) -> p h q", q=128), in0=pb[:].rearrange("p (h q) -> p h q", q=128), in1=msk[:, mi, :].unsqueeze(1).broadcast_to([128, 4, 128]), op=ALU.mult), reads=[pb, msk], writes=[pb])
                    if ii >= 1:
                        i2 = ii - 1
                        kt, rel = kts[i2]; pb = pT[i2 % 3]
                        for hh in range(4):
                            mk.op("tensor", lambda e, hh=hh, pb=pb, g=g, kt=kt, i2=i2, nk=nk: e.matmul(O[hh][:, 0:HD+1], lhsT=pb[:, hh*128:(hh+1)*128], rhs=vsb[g][:, kt, :], start=(i2 == 0), stop=(i2 == nk - 1)), reads=[pb, vsb[g]], writes=[O[hh]])
                ab = asb[qi % 2]; rb = rc[qi % 2]
                for hh in range(4):
                    if even:
                        mk.op("vector", lambda e, hh=hh, rb=rb, g=g: e.tensor_tensor(out=rb[:, hh:hh+1], in0=O[hh][:, HD:HD+1], in1=snk[:, g*4+hh:g*4+hh+1], op=ALU.add), reads=[O[hh], snk], writes=[rb])
                        mk.op("vector", lambda e, hh=hh, rb=rb: e.reciprocal(out=rb[:, hh:hh+1], in_=rb[:, hh:hh+1]), reads=[rb], writes=[rb])
                    else:
                        mk.op("vector", lambda e, hh=hh, rb=rb: e.reciprocal(out=rb[:, hh:hh+1], in_=O[hh][:, HD:HD+1]), reads=[O[hh]], writes=[rb])
                    mk.op("vector", lambda e, hh=hh, rb=rb, ab=ab: e.tensor_scalar(out=ab[:, hh*HD:(hh+1)*HD], in0=O[hh][:, 0:HD], scalar1=rb[:, hh:hh+1], scalar2=None, op0=ALU.mult), reads=[O[hh], rb], writes=[ab])
                nch = (4 * HD) // 128
                for c in range(nch):
                    mk.op("tensor", lambda e, c=c, ab=ab: e.transpose(out=TP[:, c, :], in_=ab[:, c*128:(c+1)*128], identity=idb[:]), reads=[ab, idb], writes=[TP])
                fc0 = g * nch
                mk.op("scalar", lambda e, fc0=fc0, nch=nch, ql=ql: e.copy(out=attnT[:, fc0:fc0+nch, ql*128:(ql+1)*128], in_=TP[:, 0:nch, :]), reads=[TP], writes=[attnT])
        if even:
            for tl in range(16):
                t = chk * 16 + tl
                b = t % 2
                var = 0 if t == 0 else (2 if t == 63 else 1)
                mk.dma("sync", cu[b], lambda e, b=b, t=t: e.dma_start(out=um[b][:], in_=D["Ubuf"][128*t: 128*t + 128, :]), writes=[um[b]])
                if t == 0 or t == 63:
                    mk.op("vector", lambda e, b=b: e.memset(uh[b][:], 0.0), writes=[uh[b]])
                if t > 0:
                    mk.dma("sync", cu[b], lambda e, b=b, t=t: e.dma_start(out=uh[b][0:8, :], in_=D["Ubuf"][128*t - 8: 128*t, :]), writes=[uh[b]])
                if t < 63:
                    mk.dma("sync", cu[b], lambda e, b=b, t=t: e.dma_start(out=uh[b][8:16, :], in_=D["Ubuf"][128*t + 128: 128*t + 136, :]), writes=[uh[b]])
                mk.seal(cu[b], [um[b], uh[b]])
                for gi in range(4):
                    mk.op("tensor", lambda e, b=b, gi=gi, var=var: e.matmul(S[0][:, gi*128:(gi+1)*128], lhsT=um[b][:, gi*128:(gi+1)*128], rhs=bm[:, var, gi, :], start=True, stop=False), reads=[um[b], bm], writes=[S[0]])
                    mk.op("tensor", lambda e, b=b, gi=gi: e.matmul(S[0][:, gi*128:(gi+1)*128], lhsT=uh[b][:, gi*128:(gi+1)*128], rhs=bh[:, gi, :], start=False, stop=True), reads=[uh[b], bh], writes=[S[0]])
                mk.op("vector", lambda e, b=b: e.tensor_copy(out=mT[b][:].rearrange("p g t -> p (g t)"), in_=S[0][:]), reads=[S[0]], writes=[mT[b]])
                for gi in range(4):
                    mk.op("tensor", lambda e, b=b, gi=gi: e.matmul(S[1][:, gi*128:(gi+1)*128], lhsT=wp[:, gi, :], rhs=mT[b][:, gi, :], start=True, stop=True), reads=[wp, mT[b]], writes=[S[1]])
                for gi in range(4):
                    mk.op("scalar", lambda e, gi=gi, tl=tl: e.activation(out=attnT[:, 4 + gi, tl*128:(tl+1)*128], in_=S[1][:, gi*128:(gi+1)*128], func=AF.Copy, scale=bs[:, gi:gi+1]), reads=[S[1], bs], writes=[attnT])
        for tl in range(16):
            t = chk * 16 + tl
            b = t % 2
            r0 = t * 128
            c0 = tl * 128
            mk.dma("sync", cx[b], lambda e, b=b, r0=r0: e.dma_start(out=xt[b][:], in_=x_src[r0:r0+128, :]), writes=[xt[b]])
            for nb in range(2):
                for fc in range(8):
                    mk.op("tensor", lambda e, nb=nb, fc=fc, c0=c0: e.matmul(O[nb][:], lhsT=attnT[:, fc, c0:c0+128], rhs=wo[:, fc, nb*512:(nb+1)*512], start=(fc == 0), stop=(fc == 7)), reads=[attnT, wo], writes=[O[nb]])
                mk.op("vector", lambda e, nb=nb, b=b: e.tensor_tensor(out=xt[b][:, nb*512:(nb+1)*512], in0=O[nb][:], in1=xt[b][:, nb*512:(nb+1)*512], op=ALU.add), reads=[O[nb], xt[b]], writes=[xt[b]])
            mk.dma("gpsimd", so[b], lambda e, b=b, r0=r0: e.dma_start(out=D["xbuf"][r0:r0+128, :], in_=xt[b][:]), reads=[xt[b]])
            mk.op("scalar", lambda e, b=b: e.activation(out=sq[:], in_=xt[b][:], func=AF.Square, accum_out=st[b][:, 0:1]), reads=[xt[b]], writes=[sq, st[b]])
            mk.op("scalar", lambda e, b=b: e.activation(out=st[b][:, 1:2], in_=st[b][:, 0:1], func=AF.Sqrt, scale=1.0/1024, bias=1e-6), reads=[st[b]], writes=[st[b]])
            mk.op("vector", lambda e, b=b: e.reciprocal(out=st[b][:, 2:3], in_=st[b][:, 1:2]), reads=[st[b]], writes=[st[b]])
            mk.op("vector", lambda e, b=b: e.scalar_tensor_tensor(out=h2f[b][:], in0=xt[b][:], scalar=st[b][:, 2:3], in1=gt[:], op0=ALU.mult, op1=ALU.mult), reads=[xt[b], st[b], gt], writes=[h2f[b]])
            mk.op("scalar", lambda e, b=b: e.copy(out=h2b[b][:], in_=h2f[b][:]), reads=[h2f[b]], writes=[h2b[b]])
            mk.dma("gpsimd", so[b], lambda e, b=b, r0=r0: e.dma_start(out=D["h2buf"][r0:r0+128, :], in_=h2b[b][:]), reads=[h2b[b]])
            for kc in range(8):
                dst = S[kc // 4]
                mk.op("tensor", lambda e, b=b, kc=kc, dst=dst: e.transpose(out=dst[:, (kc % 4)*128:(kc % 4 + 1)*128], in_=h2f[b][:, kc*128:(kc+1)*128], identity=idf[:]), reads=[h2f[b], idf], writes=[dst])
            for hf in range(2):
                mk.op("vector", lambda e, b=b, hf=hf: e.tensor_copy(out=h2T[b][:, hf*4:(hf+1)*4, :].rearrange("p a t -> p (a t)"), in_=S[hf][:]), reads=[S[hf]], writes=[h2T[b]])
            for kc in range(8):
                mk.op("tensor", lambda e, b=b, kc=kc: e.matmul(X[:, 0:16], lhsT=h2T[b][:, kc, :], rhs=wr[:, kc, :], start=(kc == 0), stop=(kc == 7)), reads=[h2T[b], wr], writes=[X])
            mk.op("scalar", lambda e, b=b: e.activation(out=ex[b][:], in_=X[:, 0:16], func=AF.Exp, accum_out=st[b][:, 3:4]), reads=[X], writes=[ex[b], st[b]])
            mk.op("vector", lambda e, b=b: e.reciprocal(out=st[b][:, 3:4], in_=st[b][:, 3:4]), reads=[st[b]], writes=[st[b]])
            mk.op("vector", lambda e, b=b, t=t: e.tensor_scalar(out=aff_sb[:, t, :], in0=ex[b][:], scalar1=st[b][:, 3:4], scalar2=None, op0=ALU.mult), reads=[ex[b], st[b]], writes=[aff_sb])
    mk.end_stage()


NIT = 26

def stage_C(mk, i, D, G):
    NE = 16
    mk.begin_stage()
    cc = mk.chan(); ccb = mk.chan(); csc = mk.chan()
    cgr = [mk.chan() for _ in range(2)]
    cxg = [mk.chan() for _ in range(2)]
    cwt = [mk.chan() for _ in range(2)]
    PS = G["PS"]; TP = G["TP"]
    Gp = PS[0:2]; U = PS[2:4]; Y = PS[4:7]
    idb = G["idb"]; cst = G["cst"]; iot = G["iot"]; aff_sb = G["aff_sb"]
    tri_incl, tri_excl, ones = cst[:, 0, :], cst[:, 1, :], cst[:, 2, :]
    A = aff_sb[:].rearrange("p j e -> p e j")
    cbuf = D["cbuf"]; h2buf = D["h2buf"]; xbuf = D["xbuf"]
    wg = D["moe_w_gate"]; wu = D["moe_w_up"]; wd = D["moe_w_down"]
    lo = mk.sb("lo", [128, NE], F32)
    thr = mk.sb("thr", [128, NE], F32)
    cnt = mk.sb("cnt", [128, NE], F32)
    ge = mk.sb("ge", [128, NE], F32)
    m3 = mk.sb("m3", [128, NE, 64], F32)
    mk.op("vector", lambda e: e.memset(lo[:], 0.0), writes=[lo])
    for it in range(NIT):
        step = 2.0 ** -(it + 1)
        mk.op("vector", lambda e, step=step: e.tensor_scalar(out=thr[:], in0=lo[:], scalar1=step, scalar2=None, op0=ALU.add), reads=[lo], writes=[thr])
        mk.op("vector", lambda e: e.tensor_tensor(out=m3[:], in0=A, in1=thr[:].unsqueeze(2).broadcast_to([128, NE, 64]), op=ALU.is_ge), reads=[aff_sb, thr], writes=[m3])
        mk.op("vector", lambda e: e.tensor_reduce(out=cnt[:], in_=m3[:], axis=AX.X, op=ALU.add), reads=[m3], writes=[cnt])
        mk.op("tensor", lambda e: e.matmul(Gp[0][:, 0:NE], lhsT=ones, rhs=cnt[:], start=True, stop=True), reads=[cst, cnt], writes=[Gp[0]])
        mk.op("vector", lambda e, step=step: e.tensor_scalar(out=ge[:], in0=Gp[0][:, 0:NE], scalar1=1024.0, scalar2=step, op0=ALU.is_ge, op1=ALU.mult), reads=[Gp[0]], writes=[ge])
        mk.op("vector", lambda e: e.tensor_tensor(out=lo[:], in0=lo[:], in1=ge[:], op=ALU.add), reads=[lo, ge], writes=[lo])
    crs = mk.sb("crs", [128, NE, 128], F32)
    cin = mk.sb("cin", [128, NE, 64], F32)
    tot = mk.sb("tot", [128, NE], F32)
    offx = mk.sb("offx", [128, NE], F32)
    totb = mk.sb("totb", [128, 128], F32)
    offr = mk.sb("offr", [128, NE, 128], F32)
    mk.op("vector", lambda e: e.tensor_tensor(out=m3[:], in0=A, in1=lo[:].unsqueeze(2).broadcast_to([128, NE, 64]), op=ALU.is_ge), reads=[aff_sb, lo], writes=[m3])
    mk.op("vector", lambda e: e.tensor_tensor(out=crs[:, :, 64:128], in0=A, in1=m3[:], op=ALU.mult), reads=[aff_sb, m3], writes=[crs])
    for p in range(NE):
        mk.op("vector", lambda e, p=p: e.tensor_tensor_scan(out=cin[:, p, :], data0=ones[:, 0:64], data1=m3[:, p, :], initial=0.0, op0=ALU.mult, op1=ALU.add), reads=[cst, m3], writes=[cin])
    mk.op("vector", lambda e: e.tensor_copy(out=tot[:], in_=cin[:, :, 63]), reads=[cin], writes=[tot])
    mk.op("tensor", lambda e: e.matmul(Gp[0][:, 0:NE], lhsT=tri_excl, rhs=tot[:], start=True, stop=True), reads=[cst, tot], writes=[Gp[0]])
    mk.op("vector", lambda e: e.tensor_copy(out=offx[:], in_=Gp[0][:, 0:NE]), reads=[Gp[0]], writes=[offx])
    mk.op("vector", lambda e: e.tensor_tensor(out=crs[:, :, 0:64], in0=cin[:], in1=offx[:].unsqueeze(2).broadcast_to([128, NE, 64]), op=ALU.add), reads=[cin, offx], writes=[crs])
    for p in range(NE):
        mk.op("vector", lambda e, p=p: e.tensor_copy(out=totb[:], in_=tot[:, p:p+1].broadcast_to([128, 128])), reads=[tot], writes=[totb])
        mk.op("tensor", lambda e, p=p: e.matmul(Gp[1][:, 0:128], lhsT=totb[:], rhs=tri_incl, start=True, stop=True), reads=[totb, cst], writes=[Gp[1]])
        mk.op("vector", lambda e, p=p: e.tensor_copy(out=offr[:, p, :], in_=Gp[1][:, 0:128]), reads=[Gp[1]], writes=[offr])
    Tcb = T("cbuf", cbuf)
    for q4 in range(4):
        mk.dma("sync", ccb, lambda e, q4=q4: e.dma_start(out=cbuf[q4*512:(q4+1)*512, :].rearrange("(p q) c -> q p c", q=128), in_=crs[:, q4*4:(q4+1)*4, :]), reads=[crs], writes=[Tcb])
    mk.seal(ccb, [Tcb])
    NC_ = NE * 8
    pc = mk.sb("pc", [128, NC_], F32)
    pcf = mk.sb("pcf", [128, NC_], F32)
    pci = mk.sb("pci", [128, NC_], I32)
    c2 = mk.sb("c2", [128, NC_], F32)
    gate = mk.sb("gate", [128, NC_], F32)
    tif = mk.sb("tif", [128, NC_], F32)
    tii = mk.sb("tii", [128, NC_], I32)
    junk = mk.sb("junk", [128, 128], F32)
    crow = [mk.sb(f"crow{k}", [128, 128], F32) for k in range(2)]
    for p in range(NE):
        for sb in range(8):
            col = p * 8 + sb
            mk.op("vector", lambda e, p=p, sb=sb, col=col: e.tensor_scalar(out=junk[:], in0=offr[:, p, :], scalar1=iot[:, sb:sb+1], scalar2=0.0, op0=ALU.is_le, op1=ALU.add, accum_out=pc[:, col:col+1]), reads=[offr, iot], writes=[junk, pc])
    mk.op("vector", lambda e: e.tensor_copy(out=pcf[:], in_=pc[:]), reads=[pc], writes=[pcf])
    for p in range(1, NE):
        mk.op("vector", lambda e, p=p: e.tensor_scalar(out=pcf[:, p*8:(p+1)*8], in0=pcf[:, p*8:(p+1)*8], scalar1=float(p * 128), scalar2=None, op0=ALU.add), reads=[pcf], writes=[pcf])
    mk.op("vector", lambda e: e.tensor_copy(out=pci[:], in_=pcf[:]), reads=[pcf], writes=[pci])
    for p in range(NE):
        for sb in range(8):
            col = p * 8 + sb
            cb = col % 2
            mk.dma("gpsimd", cgr[cb], lambda e, cb=cb, col=col: e.indirect_dma_start(out=crow[cb][:], out_offset=None, in_=cbuf[:, :], in_offset=bass.IndirectOffsetOnAxis(ap=pci[:, col:col+1], axis=0)), reads=[pci, Tcb], writes=[crow[cb]])
            mk.op("vector", lambda e, cb=cb, sb=sb, col=col: e.tensor_scalar(out=junk[:, 0:64], in0=crow[cb][:, 0:64], scalar1=iot[:, sb:sb+1], scalar2=0.0, op0=ALU.is_le, op1=ALU.add, accum_out=c2[:, col:col+1]), reads=[crow[cb], iot], writes=[junk, c2])
            mk.op("vector", lambda e, cb=cb, sb=sb, col=col: e.scalar_tensor_tensor(out=junk[:, 64:128], in0=crow[cb][:, 0:64], scalar=iot[:, 8+sb:9+sb], in1=crow[cb][:, 64:128], op0=ALU.is_equal, op1=ALU.mult, accum_out=gate[:, col:col+1]), reads=[crow[cb], iot], writes=[junk, gate])
    mk.op("vector", lambda e: e.scalar_tensor_tensor(out=tif[:], in0=c2[:], scalar=128.0, in1=pc[:], op0=ALU.mult, op1=ALU.add), reads=[pc, c2], writes=[tif])
    mk.op("vector", lambda e: e.tensor_copy(out=tii[:], in_=tif[:]), reads=[tif], writes=[tii])
    Th2 = T("h2buf", h2buf)
    Tx = T("xbuf", xbuf)
    xg = [mk.sb(f"xg{k}", [128, 1024], BF16) for k in range(2)]
    xsT = mk.sb("xsT", [128, 8, 1024], BF16)
    wgb = [mk.sb(f"wgb{k}", [128, 8, 256], BF16) for k in range(2)]
    wub = [mk.sb(f"wub{k}", [128, 8, 256], BF16) for k in range(2)]
    wdf = mk.sb("wdf", [128, 16, 1024], BF16)
    actF = mk.sb("actF", [128, 16, 1024], BF16)
    Tw = [T(f"wdf{k}", wdf.ap[:, 2*k:2*k+2, :]) for k in range(8)]
    Ta = [T(f"actF{k}", actF.ap[:, 2*k:2*k+2, :]) for k in range(8)]
    sg = [mk.sb(f"sg{k}", [128, 512], F32) for k in range(2)]
    yo = [mk.sb(f"yo{k}", [128, 1024], F32) for k in range(2)]
    gi_ = 0
    for p in range(NE):
        ew = i * 16 + p
        for sb in range(8):
            col = p * 8 + sb
            xb = col % 2
            mk.dma("gpsimd", cxg[xb], lambda e, xb=xb, col=col: e.indirect_dma_start(out=xg[xb][:], out_offset=None, in_=h2buf[:, :], in_offset=bass.IndirectOffsetOnAxis(ap=tii[:, col:col+1], axis=0)), reads=[tii, Th2], writes=[xg[xb]])
            for dc in range(8):
                mk.op("tensor", lambda e, xb=xb, dc=dc: e.transpose(out=TP[:, dc, :], in_=xg[xb][:, dc*128:(dc+1)*128], identity=idb[:]), reads=[xg[xb], idb], writes=[TP])
            mk.op("scalar", lambda e, sb=sb: e.copy(out=xsT[:, :, sb*128:(sb+1)*128], in_=TP[:]), reads=[TP], writes=[xsT])
        for fg in range(8):
            wb = gi_ % 2; gi_ += 1
            f0 = fg * 256
            mk.dma("gpsimd", cwt[wb], lambda e, wb=wb, ew=ew, f0=f0: e.dma_start(out=wgb[wb][:], in_=wg[ew, :, f0:f0+256].rearrange("(dc p) f -> p dc f", p=128)), writes=[wgb[wb]])
            mk.dma("gpsimd", cwt[wb], lambda e, wb=wb, ew=ew, f0=f0: e.dma_start(out=wub[wb][:], in_=wu[ew, :, f0:f0+256].rearrange("(dc p) f -> p dc f", p=128)), writes=[wub[wb]])
            mk.dma("gpsimd", cwt[wb], lambda e, fg=fg, ew=ew, f0=f0: e.dma_start(out=wdf[:, 2*fg:2*fg+2, :], in_=wd[ew, f0:f0+256, :].rearrange("(fc p) d -> p fc d", p=128)), writes=[Tw[fg]])
            mk.seal(cwt[wb], [wgb[wb], wub[wb], Tw[fg]])
            k_ = 0
            for fc in range(2):
                for tb in range(2):
                    gb = Gp[k_ % 2]; ub = U[k_ % 2]; sgb = sg[k_ % 2]; k_ += 1
                    for dc in range(8):
                        mk.op("tensor", lambda e, gb=gb, wb=wb, dc=dc, fc=fc, tb=tb: e.matmul(gb[:], lhsT=wgb[wb][:, dc, fc*128:(fc+1)*128], rhs=xsT[:, dc, tb*512:(tb+1)*512], start=(dc == 0), stop=(dc == 7)), reads=[wgb[wb], xsT], writes=[gb])
                    for dc in range(8):
                        mk.op("tensor", lambda e, ub=ub, wb=wb, dc=dc, fc=fc, tb=tb: e.matmul(ub[:], lhsT=wub[wb][:, dc, fc*128:(fc+1)*128], rhs=xsT[:, dc, tb*512:(tb+1)*512], start=(dc == 0), stop=(dc == 7)), reads=[wub[wb], xsT], writes=[ub])
                    mk.op("scalar", lambda e, gb=gb, sgb=sgb: e.activation(out=sgb[:], in_=gb[:], func=AF.Silu), reads=[gb], writes=[sgb])
                    mk.op("vector", lambda e, ub=ub, sgb=sgb, fg=fg, fc=fc, tb=tb: e.tensor_tensor(out=actF[:, 2*fg+fc, tb*512:(tb+1)*512], in0=ub[:], in1=sgb[:], op=ALU.mult), reads=[ub, sgb], writes=[Ta[fg]])
        yk = 0
        for tt in range(8):
            col = p * 8 + tt
            ob = yo[tt % 2]
            for nb in range(2):
                yb = Y[yk % 3]; yk += 1
                for fc in range(16):
                    mk.op("tensor", lambda e, yb=yb, fc=fc, tt=tt, nb=nb: e.matmul(yb[:], lhsT=actF[:, fc, tt*128:(tt+1)*128], rhs=wdf[:, fc, nb*512:(nb+1)*512], start=(fc == 0), stop=(fc == 15)), reads=[Ta[fc // 2], Tw[fc // 2]], writes=[yb])
                mk.op("vector", lambda e, yb=yb, ob=ob, nb=nb, col=col: e.tensor_scalar(out=ob[:, nb*512:(nb+1)*512], in0=yb[:], scalar1=gate[:, col:col+1], scalar2=None, op0=ALU.mult), reads=[yb, gate], writes=[ob])
            mk.dma("gpsimd", csc, lambda e, ob=ob, col=col: e.indirect_dma_start(out=xbuf[:, :], out_offset=bass.IndirectOffsetOnAxis(ap=tii[:, col:col+1], axis=0), in_=ob[:], in_offset=None, compute_op=ALU.add), reads=[ob, tii], writes=[Tx])
    mk.end_stage()


def stage_F(mk, D, G):
    mk.begin_stage()
    cc = mk.chan()
    ldx = [mk.chan() for _ in range(2)]
    sty = [mk.chan() for _ in range(2)]
    gt = mk.sb("gt", [128, 1024], F32)
    mk.dma("sync", cc, lambda e: e.dma_start(out=gt[:], in_=D["norm_final"][0:1, :].partition_broadcast(128)), writes=[gt])
    xt = [mk.sb(f"xt{k}", [128, 1024], F32) for k in range(2)]
    yt = [mk.sb(f"yt{k}", [128, 1024], F32) for k in range(2)]
    sq = mk.sb("sq", [128, 1024], F32)
    st = [mk.sb(f"st{k}", [128, 4], F32) for k in range(2)]
    final = []
    for t in range(64):
        b = t % 2
        r0 = t * 128
        mk.dma("sync", ldx[b], lambda e, b=b, r0=r0: e.dma_start(out=xt[b][:], in_=D["xbuf"][r0:r0+128, :]), writes=[xt[b]])
        mk.op("scalar", lambda e, b=b: e.activation(out=sq[:], in_=xt[b][:], func=AF.Square, accum_out=st[b][:, 0:1]), reads=[xt[b]], writes=[sq, st[b]])
        mk.op("scalar", lambda e, b=b: e.activation(out=st[b][:, 1:2], in_=st[b][:, 0:1], func=AF.Sqrt, scale=1.0/1024, bias=1e-6), reads=[st[b]], writes=[st[b]])
        mk.op("vector", lambda e, b=b: e.reciprocal(out=st[b][:, 2:3], in_=st[b][:, 1:2]), reads=[st[b]], writes=[st[b]])
        mk.op("vector", lambda e, b=b: e.scalar_tensor_tensor(out=yt[b][:], in0=xt[b][:], scalar=st[b][:, 2:3], in1=gt[:], op0=ALU.mult, op1=ALU.add if False else ALU.mult), reads=[xt[b], st[b], gt], writes=[yt[b]])
        final.append(mk.dma("gpsimd", sty[b], lambda e, b=b, r0=r0: e.dma_start(out=D["y"][r0:r0+128, :], in_=yt[b][:]), reads=[yt[b]]))
    for tok in final:
        mk.wait_tok("sync", tok)
    mk.end_stage()


def rope_cs(p, dim):
    inv = (10000.0 ** (-np.arange(0, dim, 2, dtype=np.float32) / dim)).astype(np.float32)
    ang = p.astype(np.float32)[:, None] * inv[None, :]
    return np.cos(ang).astype(np.float32), np.sin(ang).astype(np.float32)


def host_consts():
    pos = np.arange(SEQ)
    co, si = rope_cs(pos, 64)
    cs_even = np.ascontiguousarray(np.stack([co, si], 1))
    cr, sr = rope_cs(pos // 64, 64); cc, sc = rope_cs(pos % 64, 64)
    cs_odd = np.ascontiguousarray(np.stack([np.concatenate([cr, cc], 1), np.concatenate([sr, sc], 1)], 1))
    k = np.arange(128)[:, None]; q = np.arange(128)[None, :]
    masks = np.stack([(k >= q), (k <= q)], 1).astype(np.float32).astype(BF)
    Bm = np.zeros((128, 3, 4, 128), np.float32)
    Bh = np.zeros((16, 4, 128), np.float32)
    for gi, w in enumerate(POOL_WINDOWS):
        half = w // 2
        for var, t0 in enumerate((0, 1024, SEQ - 128)):
            for tl in range(128):
                Tt = t0 + tl
                lo = max(Tt - half, 0); hi = min(Tt + half, SEQ)
                cnt = hi - lo
                for s in range(lo, hi):
                    sl = s - t0
                    if 0 <= sl < 128:
                        Bm[sl, var, gi, tl] += 1.0 / cnt
                Bm[tl, var, gi, tl] -= 1.0
        for tl in range(128):
            for s in range(tl - half, tl + half):
                if s < 0:
                    Bh[s + 8, gi, tl] = 1.0 / w
                elif s >= 128:
                    Bh[8 + s - 128, gi, tl] = 1.0 / w
    cst = np.stack([(k <= q), (k < q), np.ones((128, 128)), np.eye(128)], 1).astype(np.float32)
    s = (np.arange(8)[None, :] * 128 + np.arange(128)[:, None]).astype(np.float32)
    iot = np.concatenate([s, s + 1], 1).astype(np.float32)
    return {"cs_even": cs_even, "cs_odd": cs_odd, "masks": masks, "Bm": Bm.astype(BF), "Bh": Bh.astype(BF),
            "cst": np.ascontiguousarray(cst), "iot": np.ascontiguousarray(iot)}


IN_SPECS = [
    ("x", [8192, 1024], F32), ("norm_mix", [4, 1024], F32), ("norm_ffn", [4, 1024], F32), ("norm_final", [1, 1024], F32),
    ("a_w_in", [2, 1024, 1280], F32), ("a_w_out", [2, 1024, 1024], F32), ("a_sink", [2, 8], F32),
    ("b_w_pool", [2, 4, 128, 128], F32), ("bsc", [2, 128, 4], F32), ("c_w_qkv", [2, 1024, 1536], F32),
    ("gain", [2, 1280], F32), ("c_w_out", [2, 1024, 1024], F32), ("moe_router", [4, 1024, 16], F32),
    ("moe_w_gate", [64, 1024, 2048], F32), ("moe_w_up", [64, 1024, 2048], F32), ("moe_w_down", [64, 2048, 1024], F32),
    ("cs_even", [8192, 2, 32], F32), ("cs_odd", [8192, 2, 64], F32), ("masks", [128, 2, 128], BF16),
    ("Bm", [128, 3, 4, 128], BF16), ("Bh", [16, 4, 128], BF16), ("cst", [128, 4, 128], F32), ("iot", [128, 16], F32),
]


def build_fused(depth=4):
    nc = bass.Bass("TRN2", target_bir_lowering=False)
    D = {}
    for name, shape, dt in IN_SPECS:
        D[name] = nc.dram_tensor(name, shape, dt, kind="ExternalInput").ap()
    D["y"] = nc.dram_tensor("y", [8192, 1024], F32, kind="ExternalOutput").ap()
    for name, shape, dt in [("xbuf", [8192, 1024], F32), ("h2buf", [8192, 1024], BF16), ("Ubuf", [8192, 512], BF16),
                            ("QT_o", [2, 64, 128, 512], BF16), ("QT_e", [2, 64, 64, 512], BF16),
                            ("KT_o", [2, 128, 8192], BF16), ("KT_e", [2, 64, 8192], BF16),
                            ("VA_o", [2, 64, 128, 128], BF16), ("VA_e", [2, 64, 128, 65], BF16),
                            ("cbuf", [17 * 128, 128], F32)]:
        D[name] = nc.dram_tensor(name, shape, dt).ap()
    with ExitStack() as es:
        mk = MK(nc, es)
        G = {}
        G["PS"] = [mk.ps(f"PS{k}", [128, 512], F32) for k in range(7)]
        G["TP"] = mk.ps("TP", [128, 8, 128], BF16)
        G["cst"] = mk.sb("cstg", [128, 4, 128], F32, glob=True)
        G["iot"] = mk.sb("iotg", [128, 16], F32, glob=True)
        G["idb"] = mk.sb("idb", [128, 128], BF16, glob=True)
        G["aff_sb"] = mk.sb("aff_sb", [128, 64, 16], F32, glob=True)
        G["onesb"] = mk.sb("onesb", [128, 128], BF16, glob=True)
        G["idf"] = T("idf", G["cst"].ap[:, 3, :])
        mk.begin_stage()
        c0 = mk.chan()
        mk.dma("sync", c0, lambda e: e.dma_start(out=G["cst"][:], in_=D["cst"][:]), writes=[G["cst"]])
        mk.dma("sync", c0, lambda e: e.dma_start(out=G["iot"][:], in_=D["iot"][:]), writes=[G["iot"]])
        mk.seal(c0, [G["cst"], G["iot"]])
        mk.op("vector", lambda e: e.tensor_copy(out=G["idb"][:], in_=G["cst"][:, 3, :]), reads=[G["cst"]], writes=[G["idb"]])
        mk.op("vector", lambda e: e.tensor_copy(out=G["onesb"][:], in_=G["cst"][:, 2, :]), reads=[G["cst"]], writes=[G["onesb"]])
        mk.end_stage()
        for i in range(depth):
            stage_A(mk, i, D, G)
            stage_B(mk, i, D, G)
            stage_C(mk, i, D, G)
        stage_F(mk, D, G)
    return nc


def kernel(x, norm_mix, norm_ffn, norm_final, a_w_in, a_w_out, a_sink, b_w_pool, b_scale,
           c_w_qkv, c_q_norm, c_k_norm, c_w_out, moe_router, moe_w_gate, moe_w_up, moe_w_down):
    f = lambda a: np.ascontiguousarray(np.asarray(a, dtype=np.float32))
    hc = host_consts()
    c_q_norm = f(c_q_norm); c_k_norm = f(c_k_norm); b_scale = f(b_scale)
    shared = {
        "norm_mix": f(norm_mix), "norm_ffn": f(norm_ffn), "norm_final": f(norm_final).reshape(1, 1024),
        "a_w_in": f(a_w_in), "a_w_out": f(a_w_out), "a_sink": f(a_sink), "b_w_pool": f(b_w_pool),
        "bsc": np.ascontiguousarray(b_scale.reshape(2, 4, 128).transpose(0, 2, 1)),
        "c_w_qkv": f(c_w_qkv),
        "gain": np.ascontiguousarray(np.stack([np.concatenate([np.tile(c_q_norm[j], 8), np.tile(c_k_norm[j], 2)]) for j in range(2)], 0)),
        "c_w_out": f(c_w_out), "moe_router": f(moe_router),
        "moe_w_gate": f(moe_w_gate).reshape(64, 1024, 2048), "moe_w_up": f(moe_w_up).reshape(64, 1024, 2048),
        "moe_w_down": f(moe_w_down).reshape(64, 2048, 1024),
    }
    shared.update(hc)
    x = f(x)
    nc = build_fused()
    in_maps = []
    for b in range(2):
        d = dict(shared)
        d["x"] = np.ascontiguousarray(x[b])
        in_maps.append(d)
    res = run_bass_kernel_spmd(nc, in_maps, core_ids=[0, 1])
    return np.stack([res.results[b]["y"] for b in range(2)], 0)
```
